# Optimizing a Trainium2 kernel written in Bass

```python
import math
import jax, jax.numpy as jnp
from jax import lax
import numpy as np

D_MODEL = 2048
BATCH = 4
SEQ = 2048
DEPTH = 4

ATTN_WIDTH = D_MODEL // 2
CONV_WIDTH = D_MODEL - ATTN_WIDTH
HEAD_DIM = 128
N_HEADS = ATTN_WIDTH // HEAD_DIM
IN_COLS = 3 * ATTN_WIDTH + 2 * CONV_WIDTH
BLOCK = 256
TOP_BLOCKS = 3
Q_CHUNK = 16
CONV_K = 31
N_BUCKETS = 32
MAX_DISTANCE = 128
N_GROUPS = 4
EXPERTS_PER_GROUP = 8
N_EXPERTS = N_GROUPS * EXPERTS_PER_GROUP
EXPERT_TOP_K = 2
D_EXPERT = 256
PLE_DIM = 256
ALPHA = (2 * DEPTH) ** 0.25
BETA = (8 * DEPTH) ** -0.25
LN_EPS = 1e-5

kernel_name = "hymba_moba_conformer_hmoe_deepnorm"


def layer_norm(x, g, b):
    xf = x.astype(jnp.float32)
    mu = xf.mean(-1, keepdims=True)
    var = jnp.square(xf - mu).mean(-1, keepdims=True)
    y = (xf - mu) * lax.rsqrt(var + LN_EPS) * g.astype(jnp.float32) + b.astype(jnp.float32)
    return y.astype(x.dtype)


def t5_bucket(rel):
    n = jnp.maximum(rel, 0)
    max_exact = N_BUCKETS // 2
    nf = jnp.maximum(n, max_exact).astype(jnp.float32)
    large = max_exact + (jnp.log(nf / max_exact) / math.log(MAX_DISTANCE / max_exact)
                         * (N_BUCKETS - max_exact)).astype(jnp.int32)
    large = jnp.minimum(large, N_BUCKETS - 1)
    return jnp.where(n < max_exact, n, large)


def moba_attention(q, k, v, rel_bias):
    B, S = q.shape[0], q.shape[1]
    n_blocks = -(-S // BLOCK)
    s_pad = n_blocks * BLOCK
    pad = ((0, 0), (0, s_pad - S), (0, 0), (0, 0))
    q = jnp.pad(q, pad).transpose(0, 2, 1, 3)
    k = jnp.pad(k, pad).transpose(0, 2, 1, 3)
    v = jnp.pad(v, pad).transpose(0, 2, 1, 3)
    kb = k.reshape(B, N_HEADS, n_blocks, BLOCK, HEAD_DIM)
    vb = v.reshape(B, N_HEADS, n_blocks, BLOCK, HEAD_DIM)
    k_mean = kb.astype(jnp.float32).mean(axis=3)
    n_chunks = s_pad // Q_CHUNK
    qc = q.reshape(B, N_HEADS, n_chunks, Q_CHUNK, HEAD_DIM).transpose(2, 0, 1, 3, 4)
    k_top = min(TOP_BLOCKS, n_blocks)
    scale = HEAD_DIM ** -0.5
    head_ids = jnp.arange(N_HEADS)
    block_ids = jnp.arange(n_blocks)
    gather = jax.vmap(jax.vmap(lambda blocks, idx: blocks[idx]))

    def one_chunk(args):
        q_blk, c = args
        q_pos = c * Q_CHUNK + jnp.arange(Q_CHUNK)
        cur = (c * Q_CHUNK) // BLOCK
        gate = jnp.einsum('bhqd,bhnd->bhqn', q_blk.astype(jnp.float32), k_mean)
        gate = jnp.where(block_ids < cur, gate, -jnp.inf)
        _, sel = lax.top_k(gate, k_top)
        sel_valid = sel < cur
        k_sel = gather(kb, sel)
        v_sel = gather(vb, sel)
        s_sel = jnp.einsum('bhqd,bhqkjd->bhqkj', q_blk, k_sel,
                           preferred_element_type=jnp.float32) * scale
        k_pos_sel = sel[..., None] * BLOCK + jnp.arange(BLOCK)
        bucket_sel = t5_bucket(q_pos[None, None, :, None, None] - k_pos_sel)
        s_sel = s_sel + rel_bias.T[head_ids[None, :, None, None, None], bucket_sel].astype(jnp.float32)
        s_sel = jnp.where(sel_valid[..., None], s_sel, -jnp.inf)
        k_own = lax.dynamic_index_in_dim(kb, cur, axis=2, keepdims=False)
        v_own = lax.dynamic_index_in_dim(vb, cur, axis=2, keepdims=False)
        s_own = jnp.einsum('bhqd,bhjd->bhqj', q_blk, k_own,
                           preferred_element_type=jnp.float32) * scale
        rel_own = q_pos[:, None] - (cur * BLOCK + jnp.arange(BLOCK))[None, :]
        s_own = s_own + rel_bias[t5_bucket(rel_own)].transpose(2, 0, 1)[None].astype(jnp.float32)
        s_own = jnp.where(rel_own >= 0, s_own, -jnp.inf)
        logits = jnp.concatenate([s_sel.reshape(B, N_HEADS, Q_CHUNK, k_top * BLOCK), s_own], -1)
        prob = jax.nn.softmax(logits, axis=-1)
        p_sel = prob[..., :k_top * BLOCK].reshape(B, N_HEADS, Q_CHUNK, k_top, BLOCK).astype(v.dtype)
        p_own = prob[..., k_top * BLOCK:].astype(v.dtype)
        out = (jnp.einsum('bhqkj,bhqkjd->bhqd', p_sel, v_sel)
               + jnp.einsum('bhqj,bhjd->bhqd', p_own, v_own))
        return out.astype(q.dtype)

    out = lax.map(one_chunk, (qc, jnp.arange(n_chunks)))
    out = out.transpose(1, 0, 3, 2, 4).reshape(B, s_pad, N_HEADS * HEAD_DIM)
    return out[:, :S]


def conformer_conv(a, g, conv_w, conv_b, ln_g, ln_b):
    u = a * jax.nn.sigmoid(g)
    y = lax.conv_general_dilated(u, conv_w[:, None, :].astype(u.dtype), window_strides=(1,),
                                 padding=((CONV_K - 1, 0),),
                                 dimension_numbers=('NWC', 'WIO', 'NWC'),
                                 feature_group_count=CONV_WIDTH) + conv_b
    return jax.nn.silu(layer_norm(y, ln_g, ln_b))


def hier_moe(x, wg, bg, we, be, w_gu, w_down):
    B, S, D = x.shape
    t = x.reshape(-1, D)
    T = t.shape[0]
    g_logits = (t @ wg + bg).astype(jnp.float32)
    g_prob = jax.nn.softmax(g_logits, axis=-1)
    g_top = jnp.argmax(g_logits, axis=-1)
    p_group = jnp.take_along_axis(g_prob, g_top[:, None], axis=1)[:, 0]
    e_logits = (t @ we + be).astype(jnp.float32).reshape(T, N_GROUPS, EXPERTS_PER_GROUP)
    e_in = jnp.take_along_axis(e_logits, g_top[:, None, None], axis=1)[:, 0]
    e_prob = jax.nn.softmax(e_in, axis=-1)
    top_p, top_i = lax.top_k(e_prob, EXPERT_TOP_K)
    top_p = top_p / top_p.sum(-1, keepdims=True)
    expert_ids = g_top[:, None] * EXPERTS_PER_GROUP + top_i
    weights = p_group[:, None] * top_p
    combine = (jax.nn.one_hot(expert_ids, N_EXPERTS, dtype=jnp.float32) * weights[..., None]).sum(1)
    h = jnp.einsum('td,edf->tef', t, w_gu)
    act = jax.nn.silu(h[..., :D_EXPERT]) * h[..., D_EXPERT:] * combine[..., None].astype(t.dtype)
    out = jnp.einsum('tef,efd->td', act, w_down)
    return out.reshape(B, S, D)


def setup_inputs(seed: int = 0) -> dict:
    key = jax.random.key(seed)
    ks = jax.random.split(key, 24)
    f32 = jnp.float32
    nrm = lambda k, shape, s: jax.random.normal(k, shape, f32) * s
    return {
        "x": nrm(ks[0], (BATCH, SEQ, D_MODEL), 1.0),
        "p": nrm(ks[1], (DEPTH, BATCH, SEQ, PLE_DIM), 1.0),
        "w_in": nrm(ks[2], (DEPTH, D_MODEL, IN_COLS), D_MODEL ** -0.5),
        "conv_w": nrm(ks[3], (DEPTH, CONV_K, CONV_WIDTH), CONV_K ** -0.5),
        "conv_b": nrm(ks[4], (DEPTH, CONV_WIDTH), 0.02),
        "conv_ln_g": 1.0 + nrm(ks[5], (DEPTH, CONV_WIDTH), 0.02),
        "conv_ln_b": nrm(ks[6], (DEPTH, CONV_WIDTH), 0.02),
        "w_out": nrm(ks[7], (DEPTH, ATTN_WIDTH + CONV_WIDTH, D_MODEL), BETA * D_MODEL ** -0.5),
        "rel_bias": nrm(ks[8], (N_BUCKETS, N_HEADS), 0.5),
        "ln1_g": 1.0 + nrm(ks[9], (DEPTH, D_MODEL), 0.02),
        "ln1_b": nrm(ks[10], (DEPTH, D_MODEL), 0.02),
        "router_g_w": nrm(ks[11], (DEPTH, D_MODEL, N_GROUPS), D_MODEL ** -0.5),
        "router_g_b": nrm(ks[12], (DEPTH, N_GROUPS), 0.01),
        "router_e_w": nrm(ks[13], (DEPTH, D_MODEL, N_EXPERTS), D_MODEL ** -0.5),
        "router_e_b": nrm(ks[14], (DEPTH, N_EXPERTS), 0.01),
        "expert_w_gu": nrm(ks[15], (DEPTH, N_EXPERTS, D_MODEL, 2 * D_EXPERT), D_MODEL ** -0.5),
        "expert_w_down": nrm(ks[16], (DEPTH, N_EXPERTS, D_EXPERT, D_MODEL), BETA * D_EXPERT ** -0.5),
        "ln2_g": 1.0 + nrm(ks[17], (DEPTH, D_MODEL), 0.02),
        "ln2_b": nrm(ks[18], (DEPTH, D_MODEL), 0.02),
        "ple_w": nrm(ks[19], (DEPTH, PLE_DIM, D_MODEL), 0.5 * PLE_DIM ** -0.5),
        "ple_gate_w": nrm(ks[20], (DEPTH, D_MODEL, D_MODEL), D_MODEL ** -0.5),
    }


def reference(x, p, w_in, conv_w, conv_b, conv_ln_g, conv_ln_b, w_out, rel_bias,
              ln1_g, ln1_b, router_g_w, router_g_b, router_e_w, router_e_b,
              expert_w_gu, expert_w_down, ln2_g, ln2_b, ple_w, ple_gate_w):
    B, S, _ = x.shape
    a0, a1, a2, a3 = ATTN_WIDTH, 2 * ATTN_WIDTH, 3 * ATTN_WIDTH, 3 * ATTN_WIDTH + CONV_WIDTH
    for i in range(DEPTH):
        proj = x @ w_in[i]
        q = proj[..., :a0].reshape(B, S, N_HEADS, HEAD_DIM)
        k = proj[..., a0:a1].reshape(B, S, N_HEADS, HEAD_DIM)
        v = proj[..., a1:a2].reshape(B, S, N_HEADS, HEAD_DIM)
        attn = moba_attention(q, k, v, rel_bias)
        conv = conformer_conv(proj[..., a2:a3], proj[..., a3:], conv_w[i], conv_b[i],
                              conv_ln_g[i], conv_ln_b[i])
        mix = jnp.concatenate([attn, conv], axis=-1) @ w_out[i]
        h = layer_norm(ALPHA * x + mix, ln1_g[i], ln1_b[i])
        f = hier_moe(h, router_g_w[i], router_g_b[i], router_e_w[i], router_e_b[i],
                     expert_w_gu[i], expert_w_down[i])
        h2 = layer_norm(ALPHA * h + f, ln2_g[i], ln2_b[i])
        x = h2 + jax.nn.sigmoid(h2 @ ple_gate_w[i]) * (p[i] @ ple_w[i])
    return x
```

```python
import numpy as np
import concourse.bass as bass
import concourse.mybir as mybir
from concourse.bass_utils import run_bass_kernel_spmd

F32 = mybir.dt.float32
BF16 = mybir.dt.bfloat16
AF = mybir.ActivationFunctionType
ALU = mybir.AluOpType
AX = mybir.AxisListType

NT = 2048
NH = 1024
D = 2048
DEPTH = 4
ALPHA = float((2 * DEPTH) ** 0.25)
LN_EPS = 1e-5
QSCALE = float(128 ** -0.5)
NEG = -30000.0
ENGS = ("pe", "act", "dve", "pool", "sp")

C_ID, C_ONE, C_SEL, C_CM, C_PM, C_W = 0, 128, 256, 1280, 1536, 1600


class Op:
    __slots__ = ("eng", "fn", "dma", "deps", "need", "milestone", "count", "waits", "sem", "inc")


class Sched:
    def __init__(self, nc, psem, dsems):
        self.nc = nc
        self.psem = psem
        self.dsems = dsems
        self.dsem_of = {}
        self.pcount = {e: 0 for e in ENGS}
        self.dcount = {}
        self.waited = {e: {} for e in ENGS}
        self.dyn = {}
        self.capture = None
        self.reset()

    def reset(self):
        self.ops = []
        self.last_writer = {}
        self.readers = {}

    def add(self, eng, fn, reads=(), writes=(), dma=None, inc=16):
        if self.capture is not None:
            self.capture.append((eng, fn, tuple(reads), tuple(writes), dma, inc))
            return None
        op = Op()
        op.eng, op.fn, op.dma, op.inc = eng, fn, dma, inc
        op.milestone = False
        deps = []
        for k in reads:
            w = self.last_writer.get(k)
            if w is not None:
                deps.append(w)
        for k in writes:
            w = self.last_writer.get(k)
            if w is not None:
                deps.append(w)
            deps.extend(self.readers.get(k, ()))
        op.deps = deps
        for k in writes:
            self.last_writer[k] = op
            self.readers[k] = []
        for k in reads:
            self.readers.setdefault(k, []).append(op)
        self.ops.append(op)
        return op

    def add_interleaved(self, A, B):
        def touch(lst):
            first, last, wr = {}, {}, set()
            for i, (eng, fn, rd, wrs, dma, inc) in enumerate(lst):
                for k in rd + wrs:
                    first.setdefault(k, i)
                    last[k] = i
                wr.update(wrs)
            return first, last, wr
        fa, la, wa = touch(A)
        fb, lb, wb = touch(B)
        conf = [k for k in la if k in fb and (k in wa or k in wb)]
        L = 0
        while L < len(A):
            pos_a = lambda i: i if i < L else L + 2 * (i - L)
            ok = all(pos_a(la[k]) < L + 2 * fb[k] + 1 for k in conf)
            if ok:
                break
            L += 1
        out = list(A[:L])
        ia, ib = L, 0
        while ia < len(A) or ib < len(B):
            if ia < len(A):
                out.append(A[ia]); ia += 1
            if ib < len(B):
                out.append(B[ib]); ib += 1
        for a in out:
            self.add(*a)
        return L

    def run(self):
        nc = self.nc
        ops = self.ops
        for op in ops:
            need = []
            seen = set()
            for d in op.deps:
                if d is op or id(d) in seen:
                    continue
                seen.add(id(d))
                if d.dma is not None:
                    need.append(d)
                elif d.eng == op.eng and op.dma is None and op.eng == "pe":
                    continue
                else:
                    d.milestone = True
                    need.append(d)
            op.need = need
        free = list(self.dsems)
        self.dsem_of = {}
        for op in ops:
            if op.dma is not None:
                key = (op.eng, op.dma)
                if key not in self.dsem_of:
                    self.dsem_of[key] = free.pop()
                sem = self.dsem_of[key]
                self.dcount[id(sem)] = self.dcount.get(id(sem), 0) + op.inc
                op.count = self.dcount[id(sem)]
            elif op.milestone:
                self.pcount[op.eng] += 1
                op.count = self.pcount[op.eng]
        per = {e: [] for e in ENGS}
        used_d = {e: {} for e in ENGS}
        for op in ops:
            ws = []
            wd = self.waited[op.eng]
            for d in op.need:
                if d.dma is not None:
                    sem = self.dsem_of[(d.eng, d.dma)]
                else:
                    sem = self.psem[d.eng]
                sid = id(sem)
                if wd.get(sid, (None, 0))[1] >= d.count:
                    continue
                wd[sid] = (sem, d.count)
                ws.append((sem, d.count))
            op.waits = ws
            per[op.eng].append(op)
            if op.dma is not None:
                used_d[op.eng][(op.eng, op.dma)] = op.count
        psem, dsem_of = self.psem, self.dsem_of

        def emit(name, e):
            if name in ("sp", "pool") and ("r2", name) not in self.dyn:
                self.dyn[("r2", name)] = e.snap(e.partition_id() % 2, min_val=0, max_val=1)
            for op in per[name]:
                for (sem, val) in op.waits:
                    e.wait_ge(sem, val)
                ins = op.fn(e)
                if op.dma is not None:
                    for one in (ins if isinstance(ins, list) else [ins]):
                        one.then_inc(dsem_of[(op.eng, op.dma)], op.inc)
                elif op.milestone:
                    ins.then_inc(psem[name], 1)
            for key, cnt in used_d[name].items():
                e.wait_ge(dsem_of[key], cnt)
                self.waited[name][id(dsem_of[key])] = (dsem_of[key], cnt)

        with nc.Block() as block:
            @block.tensor
            def _(e):
                emit("pe", e)

            @block.scalar
            def _(e):
                emit("act", e)

            @block.vector
            def _(e):
                emit("dve", e)

            @block.gpsimd
            def _(e):
                emit("pool", e)

            @block.sync
            def _(e):
                emit("sp", e)
        self.reset()


def build(n_layers=DEPTH, dbg=False, upto=99):
    nc = bass.Bass("TRN2", target_bir_lowering=False)

    def din(name, shape, dt=F32):
        return nc.dram_tensor(name, list(shape), dt, kind="ExternalInput").ap()

    def dscr(name, shape, dt):
        if dbg:
            return nc.dram_tensor(name, list(shape), dt, kind="ExternalOutput").ap()
        return nc.dram_tensor(name, list(shape), dt).ap()

    x_in = din("x", [NT, D])
    p_in = din("p", [DEPTH, NH, 256])
    w_in = din("w_in", [DEPTH, D, 5120])
    convw = din("convw", [DEPTH, 128, 8 * 31])
    convv = din("convv", [DEPTH, 128, 24])
    w_out = din("w_out", [DEPTH, D, D])
    biasraw = din("biasraw", [128, 4 * 256])
    c31 = din("c31", [128, 4])
    lnv = din("lnv", [DEPTH, 4, D])
    wr = din("wr", [DEPTH, D, 36])
    rb = din("rb", [DEPTH, 36])
    w_gu = din("w_gu", [DEPTH, 32, D, 512])
    w_dn = din("w_dn", [DEPTH, 32, 256, D])
    ple_w = din("ple_w", [DEPTH, 256, D])
    ple_g = din("ple_g", [DEPTH, D, D])
    cst = din("cst", [128, C_W])
    out = nc.dram_tensor("out", [NH, D], F32, kind="ExternalOutput").ap()

    XT = dscr("XT", [D, NT], BF16)
    QKVT = dscr("QKVT", [1536, NT], BF16)
    AGT = dscr("AGT", [2048, NT], F32)
    AOh = [nc.dram_tensor(f"AOh{t}", [512, NH], BF16) for t in range(2)]
    AOg = [nc.dram_tensor(f"AOg{t}", [1024, NH], BF16) for t in range(2)]
    CONVT = dscr("CONVT", [1024, NH], BF16)
    H = dscr("H", [NH, D], F32)
    HT = dscr("HT", [D, NH], BF16)
    COMB = dscr("COMB", [NH, 32], F32)
    XSh = nc.dram_tensor("XSh", [NH, D], F32)
    XTh = [nc.dram_tensor(f"XTh{c}", [512, NH], BF16) for c in range(4)]
    XTg = [nc.dram_tensor(f"XTg{c}", [1024, NH], BF16) for c in range(4)]

    def own_rows(ap2d, j):
        return lambda r2: ap2d[bass.ts(r2, NH), :][j * 128:(j + 1) * 128, :]

    def bc3(a, pat):
        return bass.AP(a.tensor, a.offset, [list(a.ap[0])] + pat)

    def bcast_rows(ap2d_row, n):
        return bass.AP(ap2d_row.tensor, ap2d_row.offset, [[0, 128], [1, n]])

    from contextlib import ExitStack
    top = ExitStack()
    with top:
        uid = [0]

        def sb(name, shape, dt, stack=top):
            uid[0] += 1
            return stack.enter_context(nc.sbuf_tensor(f"{name}_u{uid[0]}", list(shape), dt))

        pbig = top.enter_context(nc.psum_tensor("pbig", [128, 2048], F32))
        pbs = [top.enter_context(nc.psum_tensor(f"pb{i}", [128, 512], F32)) for i in range(4, 8)]
        bank = [pbig[:, i * 512:(i + 1) * 512] for i in range(4)] + [t[:, :] for t in pbs]
        pbig_bf = pbig.bitcast(BF16)
        bank_bf = [pbig_bf[:, i * 1024:(i + 1) * 1024] for i in range(4)] + [t.bitcast(BF16)[:, :] for t in pbs]
        BK = [("bank", i) for i in range(8)]

        psem = {e: top.enter_context(nc.semaphore(f"P_{e}")) for e in ENGS}
        dsems = [top.enter_context(nc.semaphore(f"D{i}")) for i in range(80)]
        S = Sched(nc, psem, dsems)
        ccsem = top.enter_context(nc.semaphore("ccsem"))
        ccn = [0]

        cstf = sb("cstf", [128, C_W], F32)
        cstb = sb("cstb", [128, C_W], BF16)
        DA = sb("DA", [128, 4 * 256], BF16)
        identb = cstb[:, C_ID:C_ID + 128]
        onesb = cstb[:, C_ONE:C_ONE + 128]
        onesf = cstf[:, C_ONE:C_ONE + 128]

        def dma(eng, out_ap, in_ap, key, reads, writes):
            def fn(e, o=out_ap, i=in_ap):
                if callable(o) or callable(i):
                    r2 = S.dyn[("r2", eng)]
                    res = []
                    with e.If(r2 == 0):
                        res.append(e.dma_start(out=(o(0) if callable(o) else o), in_=(i(0) if callable(i) else i)))
                    with e.Else():
                        res.append(e.dma_start(out=(o(1) if callable(o) else o), in_=(i(1) if callable(i) else i)))
                    return res
                return e.dma_start(out=o, in_=i)
            S.add(eng, fn, reads, writes, dma=key)

        def mm_group(lst, reads, writes):
            def fn(e, lst=lst):
                ins = None
                for (o, l, r, st, sp) in lst:
                    ins = e.matmul(o, l, r, start=st, stop=sp)
                return ins
            S.add("pe", fn, reads, writes)

        def tr_group(lst, reads, writes):
            def fn(e, lst=lst):
                ins = None
                for (o, i) in lst:
                    ins = e.transpose(o, i, identb)
                return ins
            S.add("pe", fn, list(reads) + ["cstb"], writes)

        def copy_op(eng, o, i, reads, writes):
            if eng == "act":
                S.add("act", lambda e, o=o, i=i: e.copy(o, i), reads, writes)
            else:
                S.add(eng, lambda e, o=o, i=i: e.tensor_copy(o, i), reads, writes)

        def transpose_tile(src_bf, src_key, dst, dst_key, col0, bA, bB, flip):
            for half, b in ((0, bA), (1, bB)):
                lst = [(bank_bf[b][:, k * 128:(k + 1) * 128], src_bf[:, (half * 8 + k) * 128:(half * 8 + k + 1) * 128])
                       for k in range(8)]
                tr_group(lst, [src_key], [BK[b]])
                eng = "act" if (half + flip) % 2 == 0 else "dve"
                copy_op(eng, dst[:, half * 8:half * 8 + 8, col0:col0 + 128],
                        bank_bf[b].rearrange("p (k t) -> p k t", t=128), [BK[b]], [dst_key])

        with ExitStack() as st:
            braw = sb("braw", [128, 4 * 256], F32, st)
            c31s = sb("c31s", [128, 4], F32, st)
            dma("sp", cstf[:, :], cst, "cstf", [], ["cstf"])
            dma("pool", cstb[:, :], cst, "cstb", [], ["cstb"])
            dma("sp", braw[:, :], biasraw, "braw", [], ["braw"])
            dma("sp", c31s[:, :], c31, "c31s", [], ["c31s"])
            for h in range(4):
                S.add("dve", lambda e, h=h: e.scalar_tensor_tensor(
                    out=DA[:, h * 256:(h + 1) * 256], in0=braw[:, h * 256:(h + 1) * 256],
                    scalar=c31s[:, h:h + 1], in1=cstf[:, C_CM:C_CM + 256], op0=ALU.subtract, op1=ALU.add),
                    ["braw", "c31s", "cstf"], ["DA"])
            S.run()

        def stage_x_to_xt():
            with ExitStack() as st:
                xt_ = [sb(f"s0x{i}", [128, D], F32, st) for i in range(2)]
                xb_ = [sb(f"s0b{i}", [128, D], BF16, st) for i in range(2)]
                stg = [sb(f"s0s{i}", [128, 16, 128], BF16, st) for i in range(2)]
                for t in range(16):
                    s2 = t % 2
                    dma("sp", xt_[s2][:, :], x_in[t * 128:(t + 1) * 128, :], f"s0x{s2}", [], [f"s0x{s2}"])
                    copy_op("dve" if t % 2 else "act", xb_[s2][:, :], xt_[s2][:, :], [f"s0x{s2}"], [f"s0b{s2}"])
                    transpose_tile(xb_[s2], f"s0b{s2}", stg[s2], f"s0s{s2}", 0, 4 + 2 * (t % 2), 5 + 2 * (t % 2), t)
                    dma("sp", XT.rearrange("(k p) t -> p k t", p=128)[:, :, t * 128:(t + 1) * 128],
                        stg[s2][:, :, :], f"s0s{s2}", [f"s0s{s2}"], [])
                S.run()

        def stage_inproj(l):
            with ExitStack() as st:
                xt = sb("s1xt", [128, 16, NT], BF16, st)
                wb = [sb(f"s1w{i}", [128, 16, 512], BF16, st) for i in range(3)]
                sgb = [sb(f"s1gb{i}", [128, NT], BF16, st) for i in range(3)]
                sgf = [sb(f"s1gf{i}", [128, NT], F32, st) for i in range(3)]
                xkeys = []
                for k in range(16):
                    if l == 0:
                        dma("sp", xt[:, k, :], XT[k * 128:(k + 1) * 128, :], f"s1xt{k}", [], [f"s1xt{k}"])
                        xkeys.append(f"s1xt{k}")
                    else:
                        for r in range(2):
                            dma("sp", xt[:, k, r * NH:(r + 1) * NH], XTg[k // 4].ap()[r * 512 + (k % 4) * 128:r * 512 + (k % 4 + 1) * 128, :],
                                f"s1xt{k}_{r}", [], [f"s1xt{k}_{r}"])
                            xkeys.append(f"s1xt{k}_{r}")

                BL = [((lambda r2: r2), "q", 0), ((lambda r2: 2 + r2), "k", 512), ((lambda r2: 4 + r2), "v", 1024),
                      (6, "a", 0), (7, "a", 512), (8, "g", 1024), (9, "g", 1536)]

                def load_w(bi):
                    s3 = bi % 3
                    cb = BL[bi][0]
                    for j in range(4):
                        def src(r2, j=j, cb=cb):
                            c = cb(r2) if callable(cb) else cb
                            return w_in[l, 4 * j * 128:(4 * j + 4) * 128, c * 512:(c + 1) * 512].rearrange("(k p) c -> p k c", p=128)
                        dma("pool", wb[s3][:, 4 * j:4 * j + 4, :], (src if callable(cb) else src(0)),
                            f"s1w{s3}_{j}", [], [f"s1w{s3}_{j}"])
                load_w(0)
                load_w(1)
                cnt = 0
                for bi in range(7):
                    if bi + 2 < 7:
                        load_w(bi + 2)
                    s3 = bi % 3
                    wkeys = [f"s1w{s3}_{j}" for j in range(4)]
                    kind, rbase = BL[bi][1], BL[bi][2]
                    isf = kind in ("a", "g")
                    for sub in range(4):
                        g3 = (bi * 4 + sub) % 3
                        stg = sgf[g3] if isf else sgb[g3]
                        skey = (f"s1gf{g3}" if isf else f"s1gb{g3}")
                        for tc in range(4):
                            b = cnt % 8
                            lst = [(bank[b], wb[s3][:, k, sub * 128:(sub + 1) * 128], xt[:, k, tc * 512:(tc + 1) * 512],
                                    k == 0, k == 15) for k in range(16)]
                            mm_group(lst, wkeys + xkeys, [BK[b]])
                            o = stg[:, tc * 512:(tc + 1) * 512]
                            pk = skey + f"_{tc}"
                            if kind == "q":
                                if cnt % 2:
                                    S.add("dve", lambda e, o=o, b=b: e.tensor_scalar_mul(o, bank[b], QSCALE), [BK[b]], [pk])
                                else:
                                    S.add("act", lambda e, o=o, b=b: e.mul(o, bank[b], QSCALE), [BK[b]], [pk])
                            else:
                                copy_op("dve" if cnt % 2 else "act", o, bank[b], [BK[b]], [pk])
                            cnt += 1
                        row = rbase + sub * 128
                        pks = [skey + f"_{tc}" for tc in range(4)]
                        if isf:
                            dma("sp", AGT[row:row + 128, :], stg[:, :], skey, pks, [])
                        else:
                            dma("sp", QKVT[row:row + 128, :], stg[:, :], skey, pks, [])
                S.run()

        def attn_gen(l, st):
            if True:
                qT = [sb(f"s2q{i}", [128, NT], BF16, st) for i in range(2)]
                kT = [sb(f"s2k{i}", [128, NT], BF16, st) for i in range(2)]
                vT = [sb(f"s2v{i}", [128, NT], BF16, st) for i in range(2)]
                Vh = sb("s2Vh", [128, 16, 128], BF16, st)
                ks = sb("s2ks", [128, 8], F32, st)
                kmb = sb("s2kmb", [128, 8], BF16, st)
                gm = sb("s2gm", [128, 64], F32, st)
                top8 = sb("s2top8", [128, 64], F32, st)
                mkv = sb("s2mkv", [128, 64], BF16, st)
                maskT = sb("s2maskT", [8, 1024], BF16, st)
                PT = [sb(f"s2PT{i}", [128, 256], BF16, st) for i in range(3)]
                rs = [sb(f"s2rs{i}", [128, 256], F32, st) for i in range(2)]
                ao = [sb(f"s2ao{i}", [128, NT], BF16, st) for i in range(2)]

                def load_head(h):
                    s2 = h % 2
                    dma("sp", qT[s2][:, :], QKVT[h * 128:(h + 1) * 128, :], f"s2q{s2}", [], [f"s2q{s2}"])
                    dma("sp", kT[s2][:, :], QKVT[512 + h * 128:512 + (h + 1) * 128, :], f"s2k{s2}", [], [f"s2k{s2}"])
                    dma("sp", vT[s2][:, :], QKVT[1024 + h * 128:1024 + (h + 1) * 128, :], f"s2v{s2}", [], [f"s2v{s2}"])
                load_head(0)
                ptc = 0
                stc = 0
                for h in range(4):
                    if h + 1 < 4:
                        load_head(h + 1)
                    s2 = h % 2
                    qk, kk, vk = f"s2q{s2}", f"s2k{s2}", f"s2v{s2}"
                    q_, k_, v_ = qT[s2], kT[s2], vT[s2]
                    DAh = DA[:, h * 256:(h + 1) * 256]
                    S.add("dve", lambda e, k_=k_: e.tensor_reduce(out=ks[:, :], in_=k_[:, :].rearrange("p (n j) -> p n j", j=256),
                                                                    axis=AX.X, op=ALU.add), [kk], ["s2ks"])
                    copy_op("dve", kmb[:, :], ks[:, :], ["s2ks"], ["s2kmb"])
                    for half in range(2):
                        b = 6 + half
                        lst = [(bank_bf[b][:, k * 128:(k + 1) * 128], v_[:, (half * 8 + k) * 128:(half * 8 + k + 1) * 128]) for k in range(8)]
                        tr_group(lst, [vk], [BK[b]])
                        copy_op("act" if half else "dve", Vh[:, half * 8:half * 8 + 8, :],
                                bank_bf[b].rearrange("p (k t) -> p k t", t=128), [BK[b]], ["s2Vh"])
                    lst = [(bank[6][:, i * 8:(i + 1) * 8], q_[:, (8 + i) * 128:(9 + i) * 128], kmb[:, :], True, True) for i in range(8)]
                    mm_group(lst, [qk, "s2kmb"], [BK[6]])
                    S.add("dve", lambda e: e.tensor_tensor(out=gm[:, :], in0=bank[6][:, 0:64], in1=cstf[:, C_PM:C_PM + 64], op=ALU.add),
                          [BK[6], "cstf"], ["s2gm"])
                    for i in range(8):
                        S.add("dve", lambda e, i=i: e.max(top8[:, i * 8:(i + 1) * 8], gm[:, i * 8:(i + 1) * 8]), ["s2gm"], [("s2top8", i)])
                        S.add("dve", lambda e, i=i: e.tensor_scalar(out=mkv[:, i * 8:(i + 1) * 8], in0=gm[:, i * 8:(i + 1) * 8],
                                                                     scalar1=top8[:, i * 8 + 2:i * 8 + 3], scalar2=NEG,
                                                                     op0=ALU.is_lt, op1=ALU.mult), ["s2gm", ("s2top8", i)], [("s2mkv", i)])
                    lst = [(bank_bf[7][0:8, i * 128:(i + 1) * 128], mkv[:, i * 8:(i + 1) * 8]) for i in range(8)]
                    tr_group(lst, [("s2mkv", i) for i in range(8)], [BK[7]])
                    copy_op("dve", maskT[:, :], bank_bf[7][0:8, :], [BK[7]], ["s2maskT"])
                    aoh = ao[s2]
                    aok = f"s2ao{s2}"
                    for J in range(8):
                        ob, sbk = J % 2, 2 + J % 2
                        tiles = []
                        for n in range(J):
                            for kt in (2 * n, 2 * n + 1):
                                tiles.append((kt, 0, 256, "A" if kt == 2 * J - 1 else None, n if J >= 4 else None))
                        tiles.append((2 * J, 0, 256, "DA", None))
                        tiles.append((2 * J + 1, 128, 128, "D", None))
                        nt_ = len(tiles)

                        def qk_op(i):
                            kt, qlo, qn, bias, mn = tiles[i]
                            b = 4 + (stc + i) % 2
                            lst = [(bank[b][:, qlo:qlo + qn], k_[:, kt * 128:(kt + 1) * 128],
                                    q_[:, J * 256 + qlo:J * 256 + qlo + qn], True, (bias is None and mn is None))]
                            rd = [kk, qk]
                            if mn is not None:
                                lst.append((bank[b][:, 0:256], cstb[0:8, C_SEL + mn * 128:C_SEL + (mn + 1) * 128],
                                            maskT[:, (J - 4) * 256:(J - 3) * 256], False, bias is None))
                                rd += ["s2maskT", "cstb"]
                            if bias == "A":
                                lst.append((bank[b][:, 0:128], identb, DAh[:, 128:256], False, True))
                            elif bias == "DA":
                                lst.append((bank[b][:, 0:256], identb, DAh[:, 0:256], False, True))
                            elif bias == "D":
                                lst.append((bank[b][:, 128:256], identb, DAh[:, 0:128], False, True))
                            if bias is not None:
                                rd += ["DA", "cstb"]
                            mm_group(lst, rd, [BK[b]])

                        def exp_pv(i):
                            kt, qlo, qn, bias, mn = tiles[i]
                            b = 4 + (stc + i) % 2
                            p3 = (ptc + i) % 3
                            S.add("act", lambda e, b=b, p3=p3, qlo=qlo, qn=qn: e.activation(
                                out=PT[p3][:, qlo:qlo + qn], in_=bank[b][:, qlo:qlo + qn], func=AF.Exp), [BK[b]], [f"s2PT{p3}"])
                            lst = [(bank[ob][:, qlo:qlo + qn], Vh[:, kt, :], PT[p3][:, qlo:qlo + qn], i == 0, i == nt_ - 1),
                                   (bank[sbk][:, qlo:qlo + qn], onesb, PT[p3][:, qlo:qlo + qn], i == 0, i == nt_ - 1)]
                            mm_group(lst, [f"s2PT{p3}", "s2Vh", "cstb"], [BK[ob], BK[sbk]])

                        qk_op(0)
                        for i in range(nt_):
                            if i + 1 < nt_:
                                qk_op(i + 1)
                            exp_pv(i)
                        stc += nt_
                        ptc += nt_
                        r2 = J % 2
                        S.add("dve", lambda e, r2=r2, sbk=sbk: e.reciprocal(rs[r2][:, :], bank[sbk][:, 0:256]), [BK[sbk]], [f"s2rs{r2}"])
                        S.add("dve", lambda e, r2=r2, ob=ob, J=J, aoh=aoh: e.tensor_tensor(
                            out=aoh[:, J * 256:(J + 1) * 256], in0=bank[ob][:, 0:256], in1=rs[r2][:, :], op=ALU.mult),
                            [BK[ob], f"s2rs{r2}"], [aok + f"_{J}"])
                        yield
                    for t in range(2):
                        dma("sp", AOh[t].ap()[h * 128:(h + 1) * 128, :], aoh[:, t * NH:(t + 1) * NH], aok + f"t{t}",
                            [aok + f"_{J}" for J in range(8)], [("AOh", h, t)])
                    yield

        def conv_gen(l, st):
            NC = NH
            if True:
                cw = sb("s3cw", [128, 8 * 31], F32, st)
                cv = sb("s3cv", [128, 24], F32, st)
                aT = [sb(f"s3a{i}", [128, 30 + NC], F32, st) for i in range(2)]
                gT = [sb(f"s3g{i}", [128, 30 + NC], F32, st) for i in range(2)]
                u = [sb(f"s3u{i}", [128, 30 + NC], F32, st) for i in range(2)]
                y = sb("s3y", [128, 8, NC], F32, st)
                sq = [sb(f"s3sq{i}", [128, 512], F32, st) for i in range(2)]
                mean = sb("s3mean", [128, NC], F32, st)
                rstd = sb("s3rstd", [128, NC], F32, st)
                mh = sb("s3mh", [128, 512], F32, st)
                msq = sb("s3msq", [128, 512], F32, st)
                ob = [sb(f"s3o{i}", [128, NC], BF16, st) for i in range(2)]
                dma("sp", cw[:, :], convw[l], "s3cw", [], ["s3cw"])
                dma("sp", cv[:, :], convv[l], "s3cv", [], ["s3cv"])
                S.add("pool", lambda e: e.memset(mh[:, :], -0.5), [], ["s3mh"])
                for i in range(2):
                    S.add("pool", lambda e, i=i: e.memset(aT[i][:, 0:30], 0.0), [], [f"s3a{i}"])
                    S.add("pool", lambda e, i=i: e.memset(gT[i][:, 0:30], 0.0), [], [f"s3g{i}"])

                def load_c(cc):
                    s2 = cc % 2
                    for (buf, nm, r0) in ((aT, "s3a", cc * 128), (gT, "s3g", 1024 + cc * 128)):
                        dma("sp", (lambda r2, buf=buf, s2=s2: buf[s2][:, 30:30 + NC] if r2 == 0 else buf[s2][:, 0:30 + NC]),
                            (lambda r2, r0=r0: AGT[r0:r0 + 128, 0:NC] if r2 == 0 else AGT[r0:r0 + 128, NH - 30:NT]),
                            f"{nm}{s2}", [], [f"{nm}{s2}"])
                load_c(0)
                for cc in range(8):
                    if cc + 1 < 8:
                        load_c(cc + 1)
                    s2 = cc % 2
                    S.add("act", lambda e, s2=s2: e.activation(out=gT[s2][:, :], in_=gT[s2][:, :], func=AF.Sigmoid), [f"s3g{s2}"], [f"s3g{s2}"])
                    S.add("dve", lambda e, s2=s2: e.tensor_tensor(out=u[s2][:, :], in0=aT[s2][:, :], in1=gT[s2][:, :], op=ALU.mult),
                          [f"s3a{s2}", f"s3g{s2}"], [f"s3u{s2}"])
                    yk = ("s3y", cc)
                    S.add("dve", lambda e, s2=s2, cc=cc: e.tensor_scalar(out=y[:, cc, :], in0=u[s2][:, 0:NC], scalar1=cw[:, cc * 31:cc * 31 + 1],
                                                                         scalar2=cv[:, cc:cc + 1], op0=ALU.mult, op1=ALU.add),
                          [f"s3u{s2}", "s3cw", "s3cv"], [yk])
                    for k in range(1, 31):
                        S.add("dve", lambda e, s2=s2, cc=cc, k=k: e.scalar_tensor_tensor(
                            out=y[:, cc, :], in0=u[s2][:, k:k + NC], scalar=cw[:, cc * 31 + k:cc * 31 + k + 1], in1=y[:, cc, :],
                            op0=ALU.mult, op1=ALU.add), [f"s3u{s2}", yk, "s3cw"], [yk])
                        if k % 5 == 0:
                            yield
                yield "hold"
                sqc = 0
                for tc in range(NC // 512):
                    b1, b2 = (tc % 2) * 2, (tc % 2) * 2 + 1
                    ts = slice(tc * 512, (tc + 1) * 512)
                    lst = [(bank[b1], onesf, y[:, cc, ts], cc == 0, cc == 7) for cc in range(8)]
                    mm_group(lst, [("s3y", cc) for cc in range(8)] + ["cstf"], [BK[b1]])
                    for cc in range(8):
                        q2 = sqc % 2
                        sqc += 1
                        S.add("act", lambda e, q2=q2, cc=cc, ts=ts: e.activation(out=sq[q2][:, :], in_=y[:, cc, ts], func=AF.Square),
                              [("s3y", cc)], [f"s3sq{q2}"])
                        mm_group([(bank[b2], onesf, sq[q2][:, :], cc == 0, cc == 7)], [f"s3sq{q2}", "cstf"], [BK[b2]])
                    mk, rk = ("s3mean", tc), ("s3rstd", tc)
                    S.add("dve", lambda e, ts=ts, b1=b1: e.tensor_scalar_mul(mean[:, ts], bank[b1], 1.0 / 1024.0), [BK[b1]], [mk])
                    S.add("dve", lambda e, ts=ts: e.tensor_tensor(out=msq[:, :], in0=mean[:, ts], in1=mean[:, ts], op=ALU.mult), [mk], ["s3msq"])
                    S.add("dve", lambda e, ts=ts, b2=b2: e.scalar_tensor_tensor(out=rstd[:, ts], in0=bank[b2], scalar=1.0 / 1024.0, in1=msq[:, :],
                                                                                op0=ALU.mult, op1=ALU.subtract), [BK[b2], "s3msq"], [rk])
                    S.add("dve", lambda e, ts=ts: e.tensor_scalar_add(rstd[:, ts], rstd[:, ts], LN_EPS), [rk], [rk])
                    S.add("pool", lambda e, ts=ts: e.tensor_tensor(out=rstd[:, ts], in0=rstd[:, ts], in1=mh[:, :], op=ALU.pow), [rk, "s3mh"], [rk])
                mks = [("s3mean", tc) for tc in range(NC // 512)]
                rks = [("s3rstd", tc) for tc in range(NC // 512)]
                for cc in range(8):
                    yk = ("s3y", cc)
                    o2 = cc % 2
                    S.add("dve", lambda e, cc=cc: e.tensor_tensor(out=y[:, cc, :], in0=y[:, cc, :], in1=mean[:, :], op=ALU.subtract), [yk] + mks, [yk])
                    S.add("dve", lambda e, cc=cc: e.tensor_tensor(out=y[:, cc, :], in0=y[:, cc, :], in1=rstd[:, :], op=ALU.mult), [yk] + rks, [yk])
                    S.add("act", lambda e, cc=cc, o2=o2: e.activation(out=ob[o2][:, :], in_=y[:, cc, :], func=AF.Silu,
                                                                      bias=cv[:, 16 + cc:17 + cc], scale=cv[:, 8 + cc:9 + cc]),
                          [yk, "s3cv"], [f"s3o{o2}"])
                    dma("sp", CONVT[cc * 128:(cc + 1) * 128, :], ob[o2][:, :], f"s3o{o2}", [f"s3o{o2}"], [])
                    yield

        def stage_attn_conv(l):
            with ExitStack() as st:
                ga, gc = attn_gen(l, st), conv_gen(l, st)
                da = dc = hold = False
                while not (da and dc):
                    if not da:
                        try:
                            next(ga)
                        except StopIteration:
                            da = True
                            for t in range(2):
                                S.add("pool", lambda e, t=t: e.collective_compute(
                                    "AllGather", ALU.bypass, replica_groups=[[0, 1], [2, 3], [4, 5], [6, 7]],
                                    ins=[AOh[t].ap().opt()], outs=[AOg[t].ap().opt()]),
                                    [("AOh", h, t) for h in range(4)], [], dma=f"ccao{t}", inc=1)
                    if not dc and not (hold and not da):
                        try:
                            hold = (next(gc) == "hold")
                        except StopIteration:
                            dc = True
                S.run()

        def layer_norm(tag, r_ap, rkey, hout, hkey, gbt, gkeys, wk):
            stats, mv, nmr, mhs = wk
            for c in range(4):
                S.add("dve", lambda e, c=c: e.bn_stats(stats[:, c * 6:(c + 1) * 6], r_ap[:, c * 512:(c + 1) * 512]), [rkey], [(tag, "st", c)])
            S.add("dve", lambda e: e.bn_aggr(mv[:, 0:2], stats[:, :]), [(tag, "st", c) for c in range(4)], [(tag, "mv")])
            S.add("dve", lambda e: e.tensor_scalar_add(mv[:, 2:3], mv[:, 1:2], LN_EPS), [(tag, "mv")], [(tag, "ve")])
            S.add("pool", lambda e: e.tensor_tensor(out=mv[:, 3:4], in0=mv[:, 2:3], in1=mhs[:, 0:1], op=ALU.pow), [(tag, "ve"), "mhs"], [(tag, "rstd")])
            S.add("dve", lambda e: e.scalar_tensor_tensor(out=nmr[:, 0:1], in0=mv[:, 0:1], scalar=-1.0, in1=mv[:, 3:4], op0=ALU.mult, op1=ALU.mult),
                  [(tag, "mv"), (tag, "rstd")], [(tag, "nmr")])
            S.add("act", lambda e: e.activation(out=hout, in_=r_ap, func=AF.Identity, bias=nmr[:, 0:1], scale=mv[:, 3:4]),
                  [rkey, (tag, "nmr"), (tag, "rstd")], [hkey])
            S.add("dve", lambda e: e.tensor_tensor(out=hout, in0=hout, in1=gbt[:, 0:D], op=ALU.mult), [hkey] + gkeys, [hkey])
            S.add("dve", lambda e: e.tensor_tensor(out=hout, in0=hout, in1=gbt[:, D:2 * D], op=ALU.add), [hkey] + gkeys, [hkey])

        def stage_outproj(l):
            xsrc = x_in
            with ExitStack() as st:
                wo = sb("s4wo", [128, 16, D], BF16, st)
                wrb = sb("s4wr", [128, 16, 36], BF16, st)
                gb = sb("s4gb", [128, 2 * D], F32, st)
                rbs = sb("s4rb", [128, 36], F32, st)
                ct = [sb(f"s4ct{i}", [128, 16, 512], BF16, st) for i in range(2)]
                xt_ = [sb(f"s4x{i}", [128, D], F32, st) for i in range(2)]
                r_ = [sb(f"s4r{i}", [128, D], F32, st) for i in range(2)]
                h_ = [sb(f"s4h{i}", [128, D], F32, st) for i in range(2)]
                hb = [sb(f"s4hb{i}", [128, D], BF16, st) for i in range(2)]
                hts = [sb(f"s4ht{i}", [128, 16, 128], BF16, st) for i in range(2)]
                stats2 = [sb(f"s4stats{i}", [128, 24], F32, st) for i in range(2)]
                mv2 = [sb(f"s4mv{i}", [128, 4], F32, st) for i in range(2)]
                nmr2 = [sb(f"s4nmr{i}", [128, 1], F32, st) for i in range(2)]
                mhs = sb("s4mhs", [128, 1], F32, st)
                lg2 = [sb(f"s4lg{i}", [128, 36], F32, st) for i in range(2)]
                w12 = [sb(f"s4w1{i}", [128, 64], F32, st) for i in range(2)]
                e82 = [sb(f"s4e8{i}", [128, 32], F32, st) for i in range(2)]
                caps = []
                cmb = sb("s4cmb", [128, 8 * 32], F32, st)
                for j in range(4):
                    dma("pool", wo[:, 4 * j:4 * j + 4, :], w_out[l, 4 * j * 128:(4 * j + 4) * 128, :].rearrange("(k p) c -> p k c", p=128),
                        f"s4wo{j}", [], [f"s4wo{j}"])
                wokeys = [f"s4wo{j}" for j in range(4)]
                dma("pool", wrb[:, :, :], wr[l].rearrange("(k p) c -> p k c", p=128), "s4wr", [], ["s4wr"])
                dma("sp", gb[:, 0:D], bcast_rows(lnv[l, 0:1, :], D), "s4gb0", [], ["s4gb0"])
                dma("sp", gb[:, D:2 * D], bcast_rows(lnv[l, 1:2, :], D), "s4gb1", [], ["s4gb1"])
                dma("sp", rbs[:, :], bcast_rows(rb[l:l + 1, :], 36), "s4rb", [], ["s4rb"])
                S.add("pool", lambda e: e.memset(mhs[:, :], -0.5), [], ["mhs"])
                for t in range(8):
                    c4, j = t // 4, t % 4
                    s2 = t % 2
                    c2 = c4 % 2
                    stats, mv, nmr, lg, w1, e8 = stats2[s2], mv2[s2], nmr2[s2], lg2[s2], w12[s2], e82[s2]
                    S.capture = []
                    caps.append(S.capture)
                    if j == 0:
                        dma("sp", ct[c2][:, 0:8, :],
                            (lambda r2, c4=c4: AOg[r2].ap().rearrange("(k p) t -> p k t", p=128)[:, :, c4 * 512:(c4 + 1) * 512]),
                            f"s4ct{c2}", [], [f"s4ct{c2}"])
                        dma("sp", ct[c2][:, 8:16, :], CONVT.rearrange("(k p) t -> p k t", p=128)[:, :, c4 * 512:(c4 + 1) * 512],
                            f"s4cv{c2}", [], [f"s4cv{c2}"])
                    dma("sp", xt_[s2][:, :], (own_rows(xsrc, t) if l == 0 else XSh.ap()[t * 128:(t + 1) * 128, :]), f"s4x{s2}", [], [f"s4x{s2}"])
                    lst = []
                    for cch in range(4):
                        lst += [(bank[cch], ct[c2][:, k, j * 128:(j + 1) * 128], wo[:, k, cch * 512:(cch + 1) * 512], k == 0, k == 15) for k in range(16)]
                    mm_group(lst, wokeys + [f"s4ct{c2}", f"s4cv{c2}"], [BK[0], BK[1], BK[2], BK[3]])
                    S.add("dve", lambda e, s2=s2: e.scalar_tensor_tensor(out=r_[s2][:, :], in0=xt_[s2][:, :], scalar=ALPHA, in1=pbig[:, :],
                                                                         op0=ALU.mult, op1=ALU.add),
                          [f"s4x{s2}", BK[0], BK[1], BK[2], BK[3]], [f"s4r{s2}"])
                    layer_norm(f"s4ln{s2}", r_[s2][:, :], f"s4r{s2}", h_[s2][:, :], f"s4h{s2}", gb, ["s4gb0", "s4gb1"], (stats, mv, nmr, mhs))
                    dma("sp", H[t * 128:(t + 1) * 128, :], h_[s2][:, :], f"s4h{s2}", [f"s4h{s2}"], [])
                    copy_op("act", hb[s2][:, :], h_[s2][:, :], [f"s4h{s2}"], [f"s4hb{s2}"])
                    transpose_tile(hb[s2], f"s4hb{s2}", hts[s2], f"s4ht{s2}", 0, 4, 5, t)
                    dma("sp", HT.rearrange("(k p) t -> p k t", p=128)[:, :, t * 128:(t + 1) * 128], hts[s2][:, :, :],
                        f"s4ht{s2}", [f"s4ht{s2}"], [])
                    lst = [(bank[6][:, 0:36], hts[s2][:, k, :], wrb[:, k, :], k == 0, k == 15) for k in range(16)]
                    mm_group(lst, [f"s4ht{s2}", "s4wr"], [BK[6]])
                    R = ("s4rt", s2)
                    S.add("dve", lambda e, lg=lg, w1=w1, e8=e8: e.tensor_tensor(out=lg[:, :], in0=bank[6][:, 0:36], in1=rbs[:, :], op=ALU.add), [BK[6], "s4rb"], [R])
                    S.add("dve", lambda e, lg=lg, w1=w1, e8=e8: e.tensor_reduce(out=w1[:, 0:1], in_=lg[:, 0:4], axis=AX.X, op=ALU.max), [R], [R])
                    S.add("dve", lambda e, lg=lg, w1=w1, e8=e8: e.tensor_scalar(out=w1[:, 4:8], in0=lg[:, 0:4], scalar1=w1[:, 0:1], scalar2=None, op0=ALU.is_ge), [R], [R])
                    S.add("dve", lambda e, lg=lg, w1=w1, e8=e8: e.tensor_scalar(out=w1[:, 8:12], in0=lg[:, 0:4], scalar1=w1[:, 0:1], scalar2=None, op0=ALU.subtract), [R], [R])
                    S.add("act", lambda e, lg=lg, w1=w1, e8=e8: e.activation(out=w1[:, 8:12], in_=w1[:, 8:12], func=AF.Exp, accum_out=w1[:, 1:2]), [R], [R])
                    S.add("dve", lambda e, lg=lg, w1=w1, e8=e8: e.reciprocal(w1[:, 2:3], w1[:, 1:2]), [R], [R])
                    S.add("dve", lambda e, lg=lg, w1=w1, e8=e8: e.tensor_tensor(
                        out=e8[:, :].rearrange("p (g j) -> p g j", j=8), in0=lg[:, 4:36].rearrange("p (g j) -> p g j", j=8),
                        in1=bc3(w1[:, 4:8], [[1, 4], [0, 8]]),
                        op=ALU.mult), [R], [R])
                    S.add("dve", lambda e, lg=lg, w1=w1, e8=e8: e.tensor_reduce(out=w1[:, 16:24], in_=e8[:, :].rearrange("p (g j) -> p j g", j=8), axis=AX.X, op=ALU.add), [R], [R])
                    S.add("dve", lambda e, lg=lg, w1=w1, e8=e8: e.tensor_reduce(out=w1[:, 12:13], in_=w1[:, 16:24], axis=AX.X, op=ALU.max), [R], [R])
                    S.add("dve", lambda e, lg=lg, w1=w1, e8=e8: e.tensor_scalar(out=w1[:, 24:32], in0=w1[:, 16:24], scalar1=w1[:, 12:13], scalar2=None, op0=ALU.is_ge), [R], [R])
                    S.add("dve", lambda e, lg=lg, w1=w1, e8=e8: e.scalar_tensor_tensor(out=w1[:, 32:40], in0=w1[:, 24:32], scalar=-1e30, in1=w1[:, 16:24],
                                                                  op0=ALU.mult, op1=ALU.add), [R], [R])
                    S.add("dve", lambda e, lg=lg, w1=w1, e8=e8: e.tensor_reduce(out=w1[:, 13:14], in_=w1[:, 32:40], axis=AX.X, op=ALU.max), [R], [R])
                    S.add("dve", lambda e, lg=lg, w1=w1, e8=e8: e.tensor_scalar(out=w1[:, 40:48], in0=w1[:, 32:40], scalar1=w1[:, 13:14], scalar2=None, op0=ALU.is_ge), [R], [R])
                    S.add("dve", lambda e, lg=lg, w1=w1, e8=e8: e.tensor_tensor(out=w1[:, 14:15], in0=w1[:, 13:14], in1=w1[:, 12:13], op=ALU.subtract), [R], [R])
                    S.add("act", lambda e, lg=lg, w1=w1, e8=e8: e.activation(out=w1[:, 14:15], in_=w1[:, 14:15], func=AF.Exp), [R], [R])
                    S.add("dve", lambda e, lg=lg, w1=w1, e8=e8: e.tensor_scalar_add(w1[:, 15:16], w1[:, 14:15], 1.0), [R], [R])
                    S.add("dve", lambda e, lg=lg, w1=w1, e8=e8: e.reciprocal(w1[:, 15:16], w1[:, 15:16]), [R], [R])
                    S.add("dve", lambda e, lg=lg, w1=w1, e8=e8: e.tensor_tensor(out=w1[:, 48:49], in0=w1[:, 15:16], in1=w1[:, 2:3], op=ALU.mult), [R], [R])
                    S.add("dve", lambda e, lg=lg, w1=w1, e8=e8: e.tensor_tensor(out=w1[:, 49:50], in0=w1[:, 48:49], in1=w1[:, 14:15], op=ALU.mult), [R], [R])
                    S.add("dve", lambda e, lg=lg, w1=w1, e8=e8: e.tensor_scalar(out=w1[:, 24:32], in0=w1[:, 24:32], scalar1=w1[:, 48:49], scalar2=None, op0=ALU.mult), [R], [R])
                    S.add("dve", lambda e, lg=lg, w1=w1, e8=e8: e.scalar_tensor_tensor(out=w1[:, 24:32], in0=w1[:, 40:48], scalar=w1[:, 49:50], in1=w1[:, 24:32],
                                                                  op0=ALU.mult, op1=ALU.add), [R], [R])
                    S.add("dve", lambda e, t=t, lg=lg, w1=w1, e8=e8: e.tensor_tensor(
                        out=cmb[:, t * 32:(t + 1) * 32].rearrange("p (g j) -> p g j", j=8),
                        in0=bc3(w1[:, 4:8], [[1, 4], [0, 8]]),
                        in1=bc3(w1[:, 24:32], [[0, 4], [1, 8]]),
                        op=ALU.mult), [R], [R, ("s4cmb", t)])
                    S.capture = None
                for t0 in range(0, 8, 2):
                    S.add_interleaved(caps[t0], caps[t0 + 1])

                dma("sp", COMB.rearrange("(t p) e -> p t e", p=128), cmb[:, :].rearrange("p (t e) -> p t e", e=32), "s4cmb",
                    [("s4cmb", t) for t in range(8)], [])
                S.run()

        def stage_moe(l, acc):
            t0 = 0
            with ExitStack() as st:
                hT = sb("s5hT", [128, 16, 1024], BF16, st)
                cb = sb("s5cb", [128, 8 * 32], F32, st)
                wg = [sb(f"s5wg{i}", [128, 16, 512], BF16, st) for i in range(3)]
                wd = [sb(f"s5wd{i}", [128, 2, D], BF16, st) for i in range(3)]
                sg = [sb(f"s5sg{i}", [128, 256], F32, st) for i in range(2)]
                ab = [sb(f"s5ab{i}", [128, 256], BF16, st) for i in range(2)]
                aT = [sb(f"s5aT{i}", [128, 256], BF16, st) for i in range(2)]
                for k in range(16):
                    dma("sp", hT[:, k, :], HT[k * 128:(k + 1) * 128, t0:t0 + 1024], f"s5hT{k}", [], [f"s5hT{k}"])
                hkeys = [f"s5hT{k}" for k in range(16)]
                dma("sp", cb[:, :].rearrange("p (t e) -> p t e", e=32), COMB[t0:t0 + 1024, :].rearrange("(t p) e -> p t e", p=128), "s5cb", [], ["s5cb"])
                def load_e(ex):
                    s3 = ex % 3
                    for j in range(4):
                        dma("pool", wg[s3][:, 4 * j:4 * j + 4, :], w_gu[l, ex, 4 * j * 128:(4 * j + 4) * 128, :].rearrange("(k p) c -> p k c", p=128),
                            f"s5wg{s3}_{j}", [], [f"s5wg{s3}_{j}"])
                    dma("pool", wd[s3][:, :, :], w_dn[l, ex].rearrange("(k p) c -> p k c", p=128), f"s5wd{s3}", [], [f"s5wd{s3}"])
                load_e(0)
                load_e(1)
                for j in range(8):
                    dma("sp", acc[:, j, :], H[j * 128:(j + 1) * 128, :], f"s5acc{j}", [], [("acc", j)])
                    S.add("dve", lambda e, j=j: e.tensor_scalar(out=acc[:, j, :], in0=acc[:, j, :], scalar1=ALPHA, scalar2=None, op0=ALU.mult),
                          [("acc", j)], [("acc", j)])
                for ex in range(32):
                    if ex + 2 < 32:
                        load_e(ex + 2)
                    s3 = ex % 3
                    gkeys = [f"s5wg{s3}_{j}" for j in range(4)]

                    def hid(i):
                        b = 4 + i % 2
                        lst = [(bank[b], hT[:, k, i * 128:(i + 1) * 128], wg[s3][:, k, :], k == 0, k == 15) for k in range(16)]
                        mm_group(lst, gkeys + hkeys, [BK[b]])
                        S.add("act", lambda e, b=b, i=i: e.activation(out=sg[i % 2][:, :], in_=bank[b][:, 0:256], func=AF.Silu), [BK[b]], [f"s5sg{i % 2}"])
                        S.add("dve", lambda e, b=b, i=i, ex=ex: e.scalar_tensor_tensor(
                            out=ab[i % 2][:, :], in0=bank[b][:, 256:512], scalar=cb[:, i * 32 + ex:i * 32 + ex + 1], in1=sg[i % 2][:, :],
                            op0=ALU.mult, op1=ALU.mult), [BK[b], f"s5sg{i % 2}", "s5cb"], [f"s5ab{i % 2}"])

                    def trp(j):
                        lst = [(bank_bf[6][:, (j % 2) * 256 + f * 128:(j % 2) * 256 + (f + 1) * 128], ab[j % 2][:, f * 128:(f + 1) * 128]) for f in range(2)]
                        tr_group(lst, [f"s5ab{j % 2}"], [("b6h", j % 2)])
                        copy_op("act", aT[j % 2][:, :], bank_bf[6][:, (j % 2) * 256:(j % 2) * 256 + 256], [("b6h", j % 2)], [f"s5aT{j % 2}"])

                    def down(j):
                        lst = []
                        for cch in range(4):
                            lst += [(bank[cch], aT[j % 2][:, f * 128:(f + 1) * 128], wd[s3][:, f, cch * 512:(cch + 1) * 512], f == 0, f == 1) for f in range(2)]
                        mm_group(lst, [f"s5aT{j % 2}", f"s5wd{s3}"], [BK[0], BK[1], BK[2], BK[3]])
                        S.add("dve", lambda e, j=j: e.tensor_tensor(out=acc[:, j, :], in0=acc[:, j, :], in1=pbig[:, :], op=ALU.add),
                              [("acc", j), BK[0], BK[1], BK[2], BK[3]], [("acc", j)])
                    hid(0)
                    hid(1)
                    for j in range(8):
                        trp(j)
                        if j + 2 < 8:
                            hid(j + 2)
                        down(j)
                S.run()

        def stage_ple(l, acc):
            last = (l == n_layers - 1)
            with ExitStack() as st:
                wgt = sb("s6wg", [128, 16, D], BF16, st)
                wpl = sb("s6wp", [128, 2, D], BF16, st)
                gb = sb("s6gb", [128, 2 * D], F32, st)
                h2b = sb("s6hb", [128, D], BF16, st)
                h2T = sb("s6hT", [128, 16, 128], BF16, st)
                pt_ = [sb(f"s6p{i}", [128, 256], F32, st) for i in range(2)]
                ptb = sb("s6pb", [128, 256], BF16, st)
                pT = sb("s6pT", [128, 256], BF16, st)
                sgm = sb("s6sg", [128, D], F32, st)
                xnb = sb("s6xb", [128, D], BF16, st)
                xts = [sb(f"s6xt{i}", [128, 16, 128], BF16, st) for i in range(2)]
                stats2 = [sb(f"s6stats{i}", [128, 24], F32, st) for i in range(2)]
                mv2 = [sb(f"s6mv{i}", [128, 4], F32, st) for i in range(2)]
                nmr2 = [sb(f"s6nmr{i}", [128, 1], F32, st) for i in range(2)]
                caps = []
                mhs = sb("s6mhs", [128, 1], F32, st)
                for j in range(4):
                    dma("pool", wgt[:, 4 * j:4 * j + 4, :], ple_g[l, 4 * j * 128:(4 * j + 4) * 128, :].rearrange("(k p) c -> p k c", p=128),
                        f"s6wg{j}", [], [f"s6wg{j}"])
                wgkeys = [f"s6wg{j}" for j in range(4)]
                dma("pool", wpl[:, :, :], ple_w[l].rearrange("(k p) c -> p k c", p=128), "s6wp", [], ["s6wp"])
                dma("sp", gb[:, 0:D], bcast_rows(lnv[l, 2:3, :], D), "s6gb0", [], ["s6gb0"])
                dma("sp", gb[:, D:2 * D], bcast_rows(lnv[l, 3:4, :], D), "s6gb1", [], ["s6gb1"])
                S.add("pool", lambda e: e.memset(mhs[:, :], -0.5), [], ["mhs"])
                PB4 = [BK[0], BK[1], BK[2], BK[3]]
                for j in range(8):
                    t = j
                    s2 = j % 2
                    stats, mv, nmr = stats2[s2], mv2[s2], nmr2[s2]
                    S.capture = []
                    caps.append(S.capture)
                    ak = ("acc", j)
                    at = acc[:, j, :]
                    dma("sp", pt_[s2][:, :], p_in[l, j * 128:(j + 1) * 128, :], f"s6p{s2}", [], [f"s6p{s2}"])
                    layer_norm(f"s6ln{s2}", at, ak, at, ak, gb, ["s6gb0", "s6gb1"], (stats, mv, nmr, mhs))
                    copy_op("act", h2b[:, :], at, [ak], ["s6hb"])
                    transpose_tile(h2b, "s6hb", h2T, "s6hT", 0, 4, 5, j)
                    lst = []
                    for cch in range(4):
                        lst += [(bank[cch], h2T[:, k, :], wgt[:, k, cch * 512:(cch + 1) * 512], k == 0, k == 15) for k in range(16)]
                    mm_group(lst, wgkeys + ["s6hT"], PB4)
                    S.add("act", lambda e: e.activation(out=sgm[:, :], in_=pbig[:, :], func=AF.Sigmoid), PB4, ["s6sg"])
                    copy_op("dve", ptb[:, :], pt_[s2][:, :], [f"s6p{s2}"], ["s6pb"])
                    tr_group([(bank_bf[6][:, f * 128:(f + 1) * 128], ptb[:, f * 128:(f + 1) * 128]) for f in range(2)], ["s6pb"], [BK[6]])
                    copy_op("dve", pT[:, :], bank_bf[6][:, 0:256], [BK[6]], ["s6pT"])
                    lst = []
                    for cch in range(4):
                        lst += [(bank[cch], pT[:, f * 128:(f + 1) * 128], wpl[:, f, cch * 512:(cch + 1) * 512], f == 0, f == 1) for f in range(2)]
                    mm_group(lst, ["s6pT", "s6wp"], PB4)
                    S.add("dve", lambda e: e.tensor_tensor(out=sgm[:, :], in0=sgm[:, :], in1=pbig[:, :], op=ALU.mult), ["s6sg"] + PB4, ["s6sg"])
                    S.add("dve", lambda e, at=at: e.tensor_tensor(out=at, in0=sgm[:, :], in1=at, op=ALU.add), ["s6sg", ak], [ak])
                    if last:
                        dma("sp", out[t * 128:(t + 1) * 128, :], at, "s6xo", [ak], [])
                    else:
                        dma("sp", XSh.ap()[t * 128:(t + 1) * 128, :], at, "s6xo", [ak], [])
                        copy_op("act", xnb[:, :], at, [ak], ["s6xb"])
                        transpose_tile(xnb, "s6xb", xts[s2], f"s6xt{s2}", 0, 6, 7, j)
                        for c in range(4):
                            dma("sp", XTh[c].ap().rearrange("(k p) t -> p k t", p=128)[:, :, t * 128:(t + 1) * 128], xts[s2][:, 4 * c:4 * c + 4, :],
                                f"s6xt{s2}_{c}", [f"s6xt{s2}"], [])
                    S.capture = None
                for t0 in range(0, 8, 2):
                    S.add_interleaved(caps[t0], caps[t0 + 1])
                S.run()

        stage_x_to_xt()
        for l in range(n_layers):
            if upto >= 1:
                stage_inproj(l)
            if upto >= 2:
                stage_attn_conv(l)
            if upto >= 4:
                stage_outproj(l)
            if upto >= 5:
                with nc.sbuf_tensor(f"acc_{l}", [128, 8, D], F32) as acc:
                    stage_moe(l, acc)
                    if upto >= 6:
                        stage_ple(l, acc)
                if upto >= 6 and l < n_layers - 1:
                    with nc.Block() as block:
                        @block.gpsimd
                        def _(g):
                            for (src, dst) in zip(XTh, XTg):
                                ccn[0] += 1
                                g.collective_compute("AllGather", ALU.bypass, replica_groups=[[0, 1], [2, 3], [4, 5], [6, 7]],
                                                     ins=[src.ap().opt()], outs=[dst.ap().opt()]).then_inc(ccsem)
                                g.wait_ge(ccsem, ccn[0])
    return nc


def t5_bucket_np(rel):
    n = np.maximum(rel, 0)
    nf = np.maximum(n, 16).astype(np.float32)
    large = 16 + (np.log(nf / np.float32(16)) / np.float32(np.log(128 / 16)) * np.float32(16)).astype(np.int32)
    large = np.minimum(large, 31)
    return np.where(n < 16, n, large)


def make_consts():
    c = np.zeros((128, C_W), np.float32)
    c[:, C_ID:C_ID + 128] = np.eye(128, dtype=np.float32)
    c[:, C_ONE:C_ONE + 128] = 1.0
    for n in range(8):
        c[n, C_SEL + n * 128:C_SEL + (n + 1) * 128] = 1.0
    k = np.arange(128)[:, None]
    i = np.arange(128)[None, :]
    c[:, C_CM:C_CM + 128] = np.where(i < k, NEG, 0.0)
    for ti in range(8):
        J = 4 + ti // 2
        for n in range(8):
            c[:, C_PM + ti * 8 + n] = 0.0 if n < J else -1e30
    return c


def host_inputs(inputs):
    f = lambda a: np.ascontiguousarray(np.asarray(a, dtype=np.float32))
    rel_bias = f(inputs["rel_bias"])
    k = np.arange(128)[:, None]
    i = np.arange(256)[None, :]
    bidx = t5_bucket_np(i - k)
    braw = rel_bias[bidx, :].transpose(0, 2, 1)
    c31f = np.broadcast_to(rel_bias[31:32, :], (128, 8))
    biasraw = [np.ascontiguousarray(braw[:, 4 * r:4 * r + 4, :].reshape(128, 4 * 256)) for r in range(2)]
    c31 = [np.ascontiguousarray(c31f[:, 4 * r:4 * r + 4]) for r in range(2)]
    conv_w = f(inputs["conv_w"])
    convw = np.ascontiguousarray(conv_w.reshape(4, 31, 8, 128).transpose(0, 3, 2, 1).reshape(4, 128, 8 * 31))
    cv = np.stack([f(inputs["conv_b"]), f(inputs["conv_ln_g"]), f(inputs["conv_ln_b"])], axis=1)
    convv = np.ascontiguousarray(cv.reshape(4, 3, 8, 128).transpose(0, 3, 1, 2).reshape(4, 128, 24))
    lnv = np.ascontiguousarray(np.stack([f(inputs["ln1_g"]), f(inputs["ln1_b"]), f(inputs["ln2_g"]), f(inputs["ln2_b"])], axis=1))
    wr = np.ascontiguousarray(np.concatenate([f(inputs["router_g_w"]), f(inputs["router_e_w"])], axis=2))
    rb = np.ascontiguousarray(np.concatenate([f(inputs["router_g_b"]), f(inputs["router_e_b"])], axis=1))
    shared = {
        "w_in": f(inputs["w_in"]), "convw": convw, "convv": convv, "w_out": f(inputs["w_out"]),
        "biasraw": biasraw, "c31": c31, "lnv": lnv, "wr": wr, "rb": rb,
        "w_gu": f(inputs["expert_w_gu"]), "w_dn": f(inputs["expert_w_down"]),
        "ple_w": f(inputs["ple_w"]), "ple_g": f(inputs["ple_gate_w"]), "cst": make_consts(),
    }
    return shared


_NC_CACHE = {}


def kernel(**inputs):
    x = np.asarray(inputs["x"], dtype=np.float32)
    p = np.asarray(inputs["p"], dtype=np.float32)
    B = x.shape[0]
    shared = host_inputs(inputs)
    if "nc" not in _NC_CACHE:
        _NC_CACHE["nc"] = build()
    nc = _NC_CACHE["nc"]
    in_maps = []
    for b in range(B):
        for r in range(2):
            m = dict(shared)
            m["x"] = np.ascontiguousarray(x[b])
            m["p"] = np.ascontiguousarray(p[:, b, r * NH:(r + 1) * NH])
            m["biasraw"] = shared["biasraw"][r]
            m["c31"] = shared["c31"][r]
            in_maps.append(m)
    res = run_bass_kernel_spmd(nc, in_maps, core_ids=list(range(2 * B)))
    outs = [np.asarray(r["out"], dtype=np.float32) for r in res.results]
    return np.stack([np.concatenate([outs[2 * b], outs[2 * b + 1]], axis=0) for b in range(B)], axis=0)
```

```python
import numpy as np
import concourse.bass as bass
import concourse.mybir as mybir
from concourse.bass_utils import run_bass_kernel_spmd

F32 = mybir.dt.float32
BF16 = mybir.dt.bfloat16
AF = mybir.ActivationFunctionType
ALU = mybir.AluOpType
AX = mybir.AxisListType

NT = 2048
NH = 1024
D = 2048
DEPTH = 4
ALPHA = float((2 * DEPTH) ** 0.25)
LN_EPS = 1e-5
QSCALE = float(128 ** -0.5)
NEG = -30000.0
ENGS = ("pe", "act", "dve", "pool", "sp")

C_ID, C_ONE, C_SEL, C_CM, C_PM, C_W = 0, 128, 256, 1280, 1536, 1600


class Op:
    __slots__ = ("eng", "fn", "dma", "deps", "need", "milestone", "count", "waits", "sem", "inc")


class Sched:
    def __init__(self, nc, psem, dsems):
        self.nc = nc
        self.psem = psem
        self.dsems = dsems
        self.dsem_of = {}
        self.pcount = {e: 0 for e in ENGS}
        self.dcount = {}
        self.waited = {e: {} for e in ENGS}
        self.dyn = {}
        self.capture = None
        self.reset()

    def reset(self):
        self.ops = []
        self.last_writer = {}
        self.readers = {}

    def add(self, eng, fn, reads=(), writes=(), dma=None, inc=16):
        if self.capture is not None:
            self.capture.append((eng, fn, tuple(reads), tuple(writes), dma, inc))
            return None
        op = Op()
        op.eng, op.fn, op.dma, op.inc = eng, fn, dma, inc
        op.milestone = False
        deps = []
        for k in reads:
            w = self.last_writer.get(k)
            if w is not None:
                deps.append(w)
        for k in writes:
            w = self.last_writer.get(k)
            if w is not None:
                deps.append(w)
            deps.extend(self.readers.get(k, ()))
        op.deps = deps
        for k in writes:
            self.last_writer[k] = op
            self.readers[k] = []
        for k in reads:
            self.readers.setdefault(k, []).append(op)
        self.ops.append(op)
        return op

    def add_interleaved(self, A, B):
        def touch(lst):
            first, last, wr = {}, {}, set()
            for i, (eng, fn, rd, wrs, dma, inc) in enumerate(lst):
                for k in rd + wrs:
                    first.setdefault(k, i)
                    last[k] = i
                wr.update(wrs)
            return first, last, wr
        fa, la, wa = touch(A)
        fb, lb, wb = touch(B)
        conf = [k for k in la if k in fb and (k in wa or k in wb)]
        L = 0
        while L < len(A):
            pos_a = lambda i: i if i < L else L + 2 * (i - L)
            ok = all(pos_a(la[k]) < L + 2 * fb[k] + 1 for k in conf)
            if ok:
                break
            L += 1
        out = list(A[:L])
        ia, ib = L, 0
        while ia < len(A) or ib < len(B):
            if ia < len(A):
                out.append(A[ia]); ia += 1
            if ib < len(B):
                out.append(B[ib]); ib += 1
        for a in out:
            self.add(*a)
        return L

    def run(self):
        nc = self.nc
        ops = self.ops
        for op in ops:
            need = []
            seen = set()
            for d in op.deps:
                if d is op or id(d) in seen:
                    continue
                seen.add(id(d))
                if d.dma is not None:
                    need.append(d)
                elif d.eng == op.eng and op.dma is None and op.eng == "pe":
                    continue
                else:
                    d.milestone = True
                    need.append(d)
            op.need = need
        free = list(self.dsems)
        self.dsem_of = {}
        for op in ops:
            if op.dma is not None:
                key = (op.eng, op.dma)
                if key not in self.dsem_of:
                    self.dsem_of[key] = free.pop()
                sem = self.dsem_of[key]
                self.dcount[id(sem)] = self.dcount.get(id(sem), 0) + op.inc
                op.count = self.dcount[id(sem)]
            elif op.milestone:
                self.pcount[op.eng] += 1
                op.count = self.pcount[op.eng]
        per = {e: [] for e in ENGS}
        used_d = {e: {} for e in ENGS}
        for op in ops:
            ws = []
            wd = self.waited[op.eng]
            for d in op.need:
                if d.dma is not None:
                    sem = self.dsem_of[(d.eng, d.dma)]
                else:
                    sem = self.psem[d.eng]
                sid = id(sem)
                if wd.get(sid, (None, 0))[1] >= d.count:
                    continue
                wd[sid] = (sem, d.count)
                ws.append((sem, d.count))
            op.waits = ws
            per[op.eng].append(op)
            if op.dma is not None:
                used_d[op.eng][(op.eng, op.dma)] = op.count
        psem, dsem_of = self.psem, self.dsem_of

        def emit(name, e):
            if name in ("sp", "pool") and ("r2", name) not in self.dyn:
                self.dyn[("r2", name)] = e.snap(e.partition_id() % 2, min_val=0, max_val=1)
            for op in per[name]:
                for (sem, val) in op.waits:
                    e.wait_ge(sem, val)
                ins = op.fn(e)
                if op.dma is not None:
                    for one in (ins if isinstance(ins, list) else [ins]):
                        one.then_inc(dsem_of[(op.eng, op.dma)], op.inc)
                elif op.milestone:
                    ins.then_inc(psem[name], 1)
            for key, cnt in used_d[name].items():
                e.wait_ge(dsem_of[key], cnt)
                self.waited[name][id(dsem_of[key])] = (dsem_of[key], cnt)

        with nc.Block() as block:
            @block.tensor
            def _(e):
                emit("pe", e)

            @block.scalar
            def _(e):
                emit("act", e)

            @block.vector
            def _(e):
                emit("dve", e)

            @block.gpsimd
            def _(e):
                emit("pool", e)

            @block.sync
            def _(e):
                emit("sp", e)
        self.reset()


def build(n_layers=DEPTH, dbg=False, upto=99):
    nc = bass.Bass("TRN2", target_bir_lowering=False)

    def din(name, shape, dt=F32):
        return nc.dram_tensor(name, list(shape), dt, kind="ExternalInput").ap()

    def dscr(name, shape, dt):
        if dbg:
            return nc.dram_tensor(name, list(shape), dt, kind="ExternalOutput").ap()
        return nc.dram_tensor(name, list(shape), dt).ap()

    x_in = din("x", [NT, D])
    p_in = din("p", [DEPTH, NH, 256])
    w_in = din("w_in", [DEPTH, D, 5120])
    convw = din("convw", [DEPTH, 128, 8 * 31])
    convv = din("convv", [DEPTH, 128, 24])
    w_out = din("w_out", [DEPTH, D, D])
    biasraw = din("biasraw", [128, 4 * 256])
    c31 = din("c31", [128, 4])
    lnv = din("lnv", [DEPTH, 4, D])
    wr = din("wr", [DEPTH, D, 36])
    rb = din("rb", [DEPTH, 36])
    w_gu = din("w_gu", [DEPTH, 32, D, 512])
    w_dn = din("w_dn", [DEPTH, 32, 256, D])
    ple_w = din("ple_w", [DEPTH, 256, D])
    ple_g = din("ple_g", [DEPTH, D, D])
    cst = din("cst", [128, C_W])
    out = nc.dram_tensor("out", [NH, D], F32, kind="ExternalOutput").ap()

    XT = dscr("XT", [D, NT], BF16)
    QKVT = dscr("QKVT", [1536, NT], BF16)
    AGT = dscr("AGT", [2048, NT], F32)
    AOh = [nc.dram_tensor(f"AOh{t}", [512, NH], BF16) for t in range(2)]
    AOg = [nc.dram_tensor(f"AOg{t}", [1024, NH], BF16) for t in range(2)]
    CONVT = dscr("CONVT", [1024, NH], BF16)
    H = dscr("H", [NH, D], F32)
    HT = dscr("HT", [D, NH], BF16)
    COMB = dscr("COMB", [NH, 32], F32)
    XSh = nc.dram_tensor("XSh", [NH, D], F32)
    XTh = [nc.dram_tensor(f"XTh{c}", [512, NH], BF16) for c in range(4)]
    XTg = [nc.dram_tensor(f"XTg{c}", [1024, NH], BF16) for c in range(4)]

    def own_rows(ap2d, j):
        return lambda r2: ap2d[bass.ts(r2, NH), :][j * 128:(j + 1) * 128, :]

    def bc3(a, pat):
        return bass.AP(a.tensor, a.offset, [list(a.ap[0])] + pat)

    def bcast_rows(ap2d_row, n):
        return bass.AP(ap2d_row.tensor, ap2d_row.offset, [[0, 128], [1, n]])

    from contextlib import ExitStack
    top = ExitStack()
    with top:
        uid = [0]

        def sb(name, shape, dt, stack=top):
            uid[0] += 1
            return stack.enter_context(nc.sbuf_tensor(f"{name}_u{uid[0]}", list(shape), dt))

        pbig = top.enter_context(nc.psum_tensor("pbig", [128, 2048], F32))
        pbs = [top.enter_context(nc.psum_tensor(f"pb{i}", [128, 512], F32)) for i in range(4, 8)]
        bank = [pbig[:, i * 512:(i + 1) * 512] for i in range(4)] + [t[:, :] for t in pbs]
        pbig_bf = pbig.bitcast(BF16)
        bank_bf = [pbig_bf[:, i * 1024:(i + 1) * 1024] for i in range(4)] + [t.bitcast(BF16)[:, :] for t in pbs]
        BK = [("bank", i) for i in range(8)]

        psem = {e: top.enter_context(nc.semaphore(f"P_{e}")) for e in ENGS}
        dsems = [top.enter_context(nc.semaphore(f"D{i}")) for i in range(80)]
        S = Sched(nc, psem, dsems)
        ccsem = top.enter_context(nc.semaphore("ccsem"))
        ccn = [0]

        cstf = sb("cstf", [128, C_W], F32)
        cstb = sb("cstb", [128, C_W], BF16)
        DA = sb("DA", [128, 4 * 256], BF16)
        identb = cstb[:, C_ID:C_ID + 128]
        onesb = cstb[:, C_ONE:C_ONE + 128]
        onesf = cstf[:, C_ONE:C_ONE + 128]

        def dma(eng, out_ap, in_ap, key, reads, writes):
            def fn(e, o=out_ap, i=in_ap):
                if callable(o) or callable(i):
                    r2 = S.dyn[("r2", eng)]
                    res = []
                    with e.If(r2 == 0):
                        res.append(e.dma_start(out=(o(0) if callable(o) else o), in_=(i(0) if callable(i) else i)))
                    with e.Else():
                        res.append(e.dma_start(out=(o(1) if callable(o) else o), in_=(i(1) if callable(i) else i)))
                    return res
                return e.dma_start(out=o, in_=i)
            S.add(eng, fn, reads, writes, dma=key)

        def mm_group(lst, reads, writes):
            def fn(e, lst=lst):
                ins = None
                for (o, l, r, st, sp) in lst:
                    ins = e.matmul(o, l, r, start=st, stop=sp)
                return ins
            S.add("pe", fn, reads, writes)

        def tr_group(lst, reads, writes):
            def fn(e, lst=lst):
                ins = None
                for (o, i) in lst:
                    ins = e.transpose(o, i, identb)
                return ins
            S.add("pe", fn, list(reads) + ["cstb"], writes)

        def copy_op(eng, o, i, reads, writes):
            if eng == "act":
                S.add("act", lambda e, o=o, i=i: e.copy(o, i), reads, writes)
            else:
                S.add(eng, lambda e, o=o, i=i: e.tensor_copy(o, i), reads, writes)

        def transpose_tile(src_bf, src_key, dst, dst_key, col0, bA, bB, flip):
            for half, b in ((0, bA), (1, bB)):
                lst = [(bank_bf[b][:, k * 128:(k + 1) * 128], src_bf[:, (half * 8 + k) * 128:(half * 8 + k + 1) * 128])
                       for k in range(8)]
                tr_group(lst, [src_key], [BK[b]])
                eng = "act" if (half + flip) % 2 == 0 else "dve"
                copy_op(eng, dst[:, half * 8:half * 8 + 8, col0:col0 + 128],
                        bank_bf[b].rearrange("p (k t) -> p k t", t=128), [BK[b]], [dst_key])

        with ExitStack() as st:
            braw = sb("braw", [128, 4 * 256], F32, st)
            c31s = sb("c31s", [128, 4], F32, st)
            dma("sp", cstf[:, :], cst, "cstf", [], ["cstf"])
            dma("pool", cstb[:, :], cst, "cstb", [], ["cstb"])
            dma("sp", braw[:, :], biasraw, "braw", [], ["braw"])
            dma("sp", c31s[:, :], c31, "c31s", [], ["c31s"])
            for h in range(4):
                S.add("dve", lambda e, h=h: e.scalar_tensor_tensor(
                    out=DA[:, h * 256:(h + 1) * 256], in0=braw[:, h * 256:(h + 1) * 256],
                    scalar=c31s[:, h:h + 1], in1=cstf[:, C_CM:C_CM + 256], op0=ALU.subtract, op1=ALU.add),
                    ["braw", "c31s", "cstf"], ["DA"])
            S.run()

        def stage_x_to_xt():
            with ExitStack() as st:
                xt_ = [sb(f"s0x{i}", [128, D], F32, st) for i in range(2)]
                xb_ = [sb(f"s0b{i}", [128, D], BF16, st) for i in range(2)]
                stg = [sb(f"s0s{i}", [128, 16, 128], BF16, st) for i in range(2)]
                for t in range(16):
                    s2 = t % 2
                    dma("sp", xt_[s2][:, :], x_in[t * 128:(t + 1) * 128, :], f"s0x{s2}", [], [f"s0x{s2}"])
                    copy_op("dve" if t % 2 else "act", xb_[s2][:, :], xt_[s2][:, :], [f"s0x{s2}"], [f"s0b{s2}"])
                    transpose_tile(xb_[s2], f"s0b{s2}", stg[s2], f"s0s{s2}", 0, 4 + 2 * (t % 2), 5 + 2 * (t % 2), t)
                    dma("sp", XT.rearrange("(k p) t -> p k t", p=128)[:, :, t * 128:(t + 1) * 128],
                        stg[s2][:, :, :], f"s0s{s2}", [f"s0s{s2}"], [])
                S.run()

        def stage_inproj(l):
            with ExitStack() as st:
                xt = sb("s1xt", [128, 16, NT], BF16, st)
                wb = [sb(f"s1w{i}", [128, 16, 512], BF16, st) for i in range(3)]
                sgb = [sb(f"s1gb{i}", [128, NT], BF16, st) for i in range(3)]
                sgf = [sb(f"s1gf{i}", [128, NT], F32, st) for i in range(3)]
                xkeys = []
                for k in range(16):
                    if l == 0:
                        dma("sp", xt[:, k, :], XT[k * 128:(k + 1) * 128, :], f"s1xt{k}", [], [f"s1xt{k}"])
                        xkeys.append(f"s1xt{k}")
                    else:
                        for r in range(2):
                            dma("sp", xt[:, k, r * NH:(r + 1) * NH], XTg[k // 4].ap()[r * 512 + (k % 4) * 128:r * 512 + (k % 4 + 1) * 128, :],
                                f"s1xt{k}_{r}", [], [f"s1xt{k}_{r}"])
                            xkeys.append(f"s1xt{k}_{r}")

                BL = [((lambda r2: r2), "q", 0), ((lambda r2: 2 + r2), "k", 512), ((lambda r2: 4 + r2), "v", 1024),
                      (6, "a", 0), (7, "a", 512), (8, "g", 1024), (9, "g", 1536)]

                def load_w(bi):
                    s3 = bi % 3
                    cb = BL[bi][0]
                    for j in range(4):
                        def src(r2, j=j, cb=cb):
                            c = cb(r2) if callable(cb) else cb
                            return w_in[l, 4 * j * 128:(4 * j + 4) * 128, c * 512:(c + 1) * 512].rearrange("(k p) c -> p k c", p=128)
                        dma("pool", wb[s3][:, 4 * j:4 * j + 4, :], (src if callable(cb) else src(0)),
                            f"s1w{s3}_{j}", [], [f"s1w{s3}_{j}"])
                load_w(0)
                load_w(1)
                cnt = 0
                for bi in range(7):
                    if bi + 2 < 7:
                        load_w(bi + 2)
                    s3 = bi % 3
                    wkeys = [f"s1w{s3}_{j}" for j in range(4)]
                    kind, rbase = BL[bi][1], BL[bi][2]
                    isf = kind in ("a", "g")
                    for sub in range(4):
                        g3 = (bi * 4 + sub) % 3
                        stg = sgf[g3] if isf else sgb[g3]
                        skey = (f"s1gf{g3}" if isf else f"s1gb{g3}")
                        for tc in range(4):
                            b = cnt % 8
                            lst = [(bank[b], wb[s3][:, k, sub * 128:(sub + 1) * 128], xt[:, k, tc * 512:(tc + 1) * 512],
                                    k == 0, k == 15) for k in range(16)]
                            mm_group(lst, wkeys + xkeys, [BK[b]])
                            o = stg[:, tc * 512:(tc + 1) * 512]
                            pk = skey + f"_{tc}"
                            if kind == "q":
                                if cnt % 2:
                                    S.add("dve", lambda e, o=o, b=b: e.tensor_scalar_mul(o, bank[b], QSCALE), [BK[b]], [pk])
                                else:
                                    S.add("act", lambda e, o=o, b=b: e.mul(o, bank[b], QSCALE), [BK[b]], [pk])
                            else:
                                copy_op("dve" if cnt % 2 else "act", o, bank[b], [BK[b]], [pk])
                            cnt += 1
                        row = rbase + sub * 128
                        pks = [skey + f"_{tc}" for tc in range(4)]
                        if isf:
                            dma("sp", AGT[row:row + 128, :], stg[:, :], skey, pks, [])
                        else:
                            dma("sp", QKVT[row:row + 128, :], stg[:, :], skey, pks, [])
                S.run()

        def attn_gen(l, st):
            if True:
                qT = [sb(f"s2q{i}", [128, NT], BF16, st) for i in range(2)]
                kT = [sb(f"s2k{i}", [128, NT], BF16, st) for i in range(2)]
                vT = [sb(f"s2v{i}", [128, NT], BF16, st) for i in range(2)]
                Vh = sb("s2Vh", [128, 16, 128], BF16, st)
                ks = sb("s2ks", [128, 8], F32, st)
                kmb = sb("s2kmb", [128, 8], BF16, st)
                gm = sb("s2gm", [128, 64], F32, st)
                top8 = sb("s2top8", [128, 64], F32, st)
                mkv = sb("s2mkv", [128, 64], BF16, st)
                maskT = sb("s2maskT", [8, 1024], BF16, st)
                PT = [sb(f"s2PT{i}", [128, 256], BF16, st) for i in range(3)]
                rs = [sb(f"s2rs{i}", [128, 256], F32, st) for i in range(2)]
                ao = [sb(f"s2ao{i}", [128, NT], BF16, st) for i in range(2)]

                def load_head(h):
                    s2 = h % 2
                    dma("sp", qT[s2][:, :], QKVT[h * 128:(h + 1) * 128, :], f"s2q{s2}", [], [f"s2q{s2}"])
                    dma("sp", kT[s2][:, :], QKVT[512 + h * 128:512 + (h + 1) * 128, :], f"s2k{s2}", [], [f"s2k{s2}"])
                    dma("sp", vT[s2][:, :], QKVT[1024 + h * 128:1024 + (h + 1) * 128, :], f"s2v{s2}", [], [f"s2v{s2}"])
                load_head(0)
                ptc = 0
                stc = 0
                for h in range(4):
                    if h + 1 < 4:
                        load_head(h + 1)
                    s2 = h % 2
                    qk, kk, vk = f"s2q{s2}", f"s2k{s2}", f"s2v{s2}"
                    q_, k_, v_ = qT[s2], kT[s2], vT[s2]
                    DAh = DA[:, h * 256:(h + 1) * 256]
                    S.add("dve", lambda e, k_=k_: e.tensor_reduce(out=ks[:, :], in_=k_[:, :].rearrange("p (n j) -> p n j", j=256),
                                                                    axis=AX.X, op=ALU.add), [kk], ["s2ks"])
                    copy_op("dve", kmb[:, :], ks[:, :], ["s2ks"], ["s2kmb"])
                    for half in range(2):
                        b = 6 + half
                        lst = [(bank_bf[b][:, k * 128:(k + 1) * 128], v_[:, (half * 8 + k) * 128:(half * 8 + k + 1) * 128]) for k in range(8)]
                        tr_group(lst, [vk], [BK[b]])
                        copy_op("act" if half else "dve", Vh[:, half * 8:half * 8 + 8, :],
                                bank_bf[b].rearrange("p (k t) -> p k t", t=128), [BK[b]], ["s2Vh"])
                    lst = [(bank[6][:, i * 8:(i + 1) * 8], q_[:, (8 + i) * 128:(9 + i) * 128], kmb[:, :], True, True) for i in range(8)]
                    mm_group(lst, [qk, "s2kmb"], [BK[6]])
                    S.add("dve", lambda e: e.tensor_tensor(out=gm[:, :], in0=bank[6][:, 0:64], in1=cstf[:, C_PM:C_PM + 64], op=ALU.add),
                          [BK[6], "cstf"], ["s2gm"])
                    for i in range(8):
                        S.add("dve", lambda e, i=i: e.max(top8[:, i * 8:(i + 1) * 8], gm[:, i * 8:(i + 1) * 8]), ["s2gm"], [("s2top8", i)])
                        S.add("dve", lambda e, i=i: e.tensor_scalar(out=mkv[:, i * 8:(i + 1) * 8], in0=gm[:, i * 8:(i + 1) * 8],
                                                                     scalar1=top8[:, i * 8 + 2:i * 8 + 3], scalar2=NEG,
                                                                     op0=ALU.is_lt, op1=ALU.mult), ["s2gm", ("s2top8", i)], [("s2mkv", i)])
                    lst = [(bank_bf[7][0:8, i * 128:(i + 1) * 128], mkv[:, i * 8:(i + 1) * 8]) for i in range(8)]
                    tr_group(lst, [("s2mkv", i) for i in range(8)], [BK[7]])
                    copy_op("dve", maskT[:, :], bank_bf[7][0:8, :], [BK[7]], ["s2maskT"])
                    aoh = ao[s2]
                    aok = f"s2ao{s2}"
                    for J in range(8):
                        ob, sbk = J % 2, 2 + J % 2
                        tiles = []
                        for n in range(J):
                            for kt in (2 * n, 2 * n + 1):
                                tiles.append((kt, 0, 256, "A" if kt == 2 * J - 1 else None, n if J >= 4 else None))
                        tiles.append((2 * J, 0, 256, "DA", None))
                        tiles.append((2 * J + 1, 128, 128, "D", None))
                        nt_ = len(tiles)

                        def qk_op(i):
                            kt, qlo, qn, bias, mn = tiles[i]
                            b = 4 + (stc + i) % 2
                            lst = [(bank[b][:, qlo:qlo + qn], k_[:, kt * 128:(kt + 1) * 128],
                                    q_[:, J * 256 + qlo:J * 256 + qlo + qn], True, (bias is None and mn is None))]
                            rd = [kk, qk]
                            if mn is not None:
                                lst.append((bank[b][:, 0:256], cstb[0:8, C_SEL + mn * 128:C_SEL + (mn + 1) * 128],
                                            maskT[:, (J - 4) * 256:(J - 3) * 256], False, bias is None))
                                rd += ["s2maskT", "cstb"]
                            if bias == "A":
                                lst.append((bank[b][:, 0:128], identb, DAh[:, 128:256], False, True))
                            elif bias == "DA":
                                lst.append((bank[b][:, 0:256], identb, DAh[:, 0:256], False, True))
                            elif bias == "D":
                                lst.append((bank[b][:, 128:256], identb, DAh[:, 0:128], False, True))
                            if bias is not None:
                                rd += ["DA", "cstb"]
                            mm_group(lst, rd, [BK[b]])

                        def exp_pv(i):
                            kt, qlo, qn, bias, mn = tiles[i]
                            b = 4 + (stc + i) % 2
                            p3 = (ptc + i) % 3
                            S.add("act", lambda e, b=b, p3=p3, qlo=qlo, qn=qn: e.activation(
                                out=PT[p3][:, qlo:qlo + qn], in_=bank[b][:, qlo:qlo + qn], func=AF.Exp), [BK[b]], [f"s2PT{p3}"])
                            lst = [(bank[ob][:, qlo:qlo + qn], Vh[:, kt, :], PT[p3][:, qlo:qlo + qn], i == 0, i == nt_ - 1),
                                   (bank[sbk][:, qlo:qlo + qn], onesb, PT[p3][:, qlo:qlo + qn], i == 0, i == nt_ - 1)]
                            mm_group(lst, [f"s2PT{p3}", "s2Vh", "cstb"], [BK[ob], BK[sbk]])

                        qk_op(0)
                        for i in range(nt_):
                            if i + 1 < nt_:
                                qk_op(i + 1)
                            exp_pv(i)
                        stc += nt_
                        ptc += nt_
                        r2 = J % 2
                        S.add("dve", lambda e, r2=r2, sbk=sbk: e.reciprocal(rs[r2][:, :], bank[sbk][:, 0:256]), [BK[sbk]], [f"s2rs{r2}"])
                        S.add("dve", lambda e, r2=r2, ob=ob, J=J, aoh=aoh: e.tensor_tensor(
                            out=aoh[:, J * 256:(J + 1) * 256], in0=bank[ob][:, 0:256], in1=rs[r2][:, :], op=ALU.mult),
                            [BK[ob], f"s2rs{r2}"], [aok + f"_{J}"])
                        yield
                    for t in range(2):
                        dma("sp", AOh[t].ap()[h * 128:(h + 1) * 128, :], aoh[:, t * NH:(t + 1) * NH], aok + f"t{t}",
                            [aok + f"_{J}" for J in range(8)], [("AOh", h, t)])
                    yield

        def conv_gen(l, st):
            NC = NH
            if True:
                cw = sb("s3cw", [128, 8 * 31], F32, st)
                cv = sb("s3cv", [128, 24], F32, st)
                aT = [sb(f"s3a{i}", [128, 30 + NC], F32, st) for i in range(2)]
                gT = [sb(f"s3g{i}", [128, 30 + NC], F32, st) for i in range(2)]
                u = [sb(f"s3u{i}", [128, 30 + NC], F32, st) for i in range(2)]
                y = sb("s3y", [128, 8, NC], F32, st)
                sq = [sb(f"s3sq{i}", [128, 512], F32, st) for i in range(2)]
                mean = sb("s3mean", [128, NC], F32, st)
                rstd = sb("s3rstd", [128, NC], F32, st)
                mh = sb("s3mh", [128, 512], F32, st)
                msq = sb("s3msq", [128, 512], F32, st)
                ob = [sb(f"s3o{i}", [128, NC], BF16, st) for i in range(2)]
                dma("sp", cw[:, :], convw[l], "s3cw", [], ["s3cw"])
                dma("sp", cv[:, :], convv[l], "s3cv", [], ["s3cv"])
                S.add("pool", lambda e: e.memset(mh[:, :], -0.5), [], ["s3mh"])
                for i in range(2):
                    S.add("pool", lambda e, i=i: e.memset(aT[i][:, 0:30], 0.0), [], [f"s3a{i}"])
                    S.add("pool", lambda e, i=i: e.memset(gT[i][:, 0:30], 0.0), [], [f"s3g{i}"])

                def load_c(cc):
                    s2 = cc % 2
                    for (buf, nm, r0) in ((aT, "s3a", cc * 128), (gT, "s3g", 1024 + cc * 128)):
                        dma("sp", (lambda r2, buf=buf, s2=s2: buf[s2][:, 30:30 + NC] if r2 == 0 else buf[s2][:, 0:30 + NC]),
                            (lambda r2, r0=r0: AGT[r0:r0 + 128, 0:NC] if r2 == 0 else AGT[r0:r0 + 128, NH - 30:NT]),
                            f"{nm}{s2}", [], [f"{nm}{s2}"])
                load_c(0)
                for cc in range(8):
                    if cc + 1 < 8:
                        load_c(cc + 1)
                    s2 = cc % 2
                    S.add("act", lambda e, s2=s2: e.activation(out=gT[s2][:, :], in_=gT[s2][:, :], func=AF.Sigmoid), [f"s3g{s2}"], [f"s3g{s2}"])
                    S.add("dve", lambda e, s2=s2: e.tensor_tensor(out=u[s2][:, :], in0=aT[s2][:, :], in1=gT[s2][:, :], op=ALU.mult),
                          [f"s3a{s2}", f"s3g{s2}"], [f"s3u{s2}"])
                    yk = ("s3y", cc)
                    S.add("dve", lambda e, s2=s2, cc=cc: e.tensor_scalar(out=y[:, cc, :], in0=u[s2][:, 0:NC], scalar1=cw[:, cc * 31:cc * 31 + 1],
                                                                         scalar2=cv[:, cc:cc + 1], op0=ALU.mult, op1=ALU.add),
                          [f"s3u{s2}", "s3cw", "s3cv"], [yk])
                    for k in range(1, 31):
                        S.add("dve", lambda e, s2=s2, cc=cc, k=k: e.scalar_tensor_tensor(
                            out=y[:, cc, :], in0=u[s2][:, k:k + NC], scalar=cw[:, cc * 31 + k:cc * 31 + k + 1], in1=y[:, cc, :],
                            op0=ALU.mult, op1=ALU.add), [f"s3u{s2}", yk, "s3cw"], [yk])
                        if k % 5 == 0:
                            yield
                yield "hold"
                sqc = 0
                for tc in range(NC // 512):
                    b1, b2 = (tc % 2) * 2, (tc % 2) * 2 + 1
                    ts = slice(tc * 512, (tc + 1) * 512)
                    lst = [(bank[b1], onesf, y[:, cc, ts], cc == 0, cc == 7) for cc in range(8)]
                    mm_group(lst, [("s3y", cc) for cc in range(8)] + ["cstf"], [BK[b1]])
                    for cc in range(8):
                        q2 = sqc % 2
                        sqc += 1
                        S.add("act", lambda e, q2=q2, cc=cc, ts=ts: e.activation(out=sq[q2][:, :], in_=y[:, cc, ts], func=AF.Square),
                              [("s3y", cc)], [f"s3sq{q2}"])
                        mm_group([(bank[b2], onesf, sq[q2][:, :], cc == 0, cc == 7)], [f"s3sq{q2}", "cstf"], [BK[b2]])
                    mk, rk = ("s3mean", tc), ("s3rstd", tc)
                    S.add("dve", lambda e, ts=ts, b1=b1: e.tensor_scalar_mul(mean[:, ts], bank[b1], 1.0 / 1024.0), [BK[b1]], [mk])
                    S.add("dve", lambda e, ts=ts: e.tensor_tensor(out=msq[:, :], in0=mean[:, ts], in1=mean[:, ts], op=ALU.mult), [mk], ["s3msq"])
                    S.add("dve", lambda e, ts=ts, b2=b2: e.scalar_tensor_tensor(out=rstd[:, ts], in0=bank[b2], scalar=1.0 / 1024.0, in1=msq[:, :],
                                                                                op0=ALU.mult, op1=ALU.subtract), [BK[b2], "s3msq"], [rk])
                    S.add("dve", lambda e, ts=ts: e.tensor_scalar_add(rstd[:, ts], rstd[:, ts], LN_EPS), [rk], [rk])
                    S.add("pool", lambda e, ts=ts: e.tensor_tensor(out=rstd[:, ts], in0=rstd[:, ts], in1=mh[:, :], op=ALU.pow), [rk, "s3mh"], [rk])
                mks = [("s3mean", tc) for tc in range(NC // 512)]
                rks = [("s3rstd", tc) for tc in range(NC // 512)]
                for cc in range(8):
                    yk = ("s3y", cc)
                    o2 = cc % 2
                    S.add("dve", lambda e, cc=cc: e.tensor_tensor(out=y[:, cc, :], in0=y[:, cc, :], in1=mean[:, :], op=ALU.subtract), [yk] + mks, [yk])
                    S.add("dve", lambda e, cc=cc: e.tensor_tensor(out=y[:, cc, :], in0=y[:, cc, :], in1=rstd[:, :], op=ALU.mult), [yk] + rks, [yk])
                    S.add("act", lambda e, cc=cc, o2=o2: e.activation(out=ob[o2][:, :], in_=y[:, cc, :], func=AF.Silu,
                                                                      bias=cv[:, 16 + cc:17 + cc], scale=cv[:, 8 + cc:9 + cc]),
                          [yk, "s3cv"], [f"s3o{o2}"])
                    dma("sp", CONVT[cc * 128:(cc + 1) * 128, :], ob[o2][:, :], f"s3o{o2}", [f"s3o{o2}"], [])
                    yield

        def stage_attn_conv(l):
            with ExitStack() as st:
                ga, gc = attn_gen(l, st), conv_gen(l, st)
                da = dc = hold = False
                while not (da and dc):
                    if not da:
                        try:
                            next(ga)
                        except StopIteration:
                            da = True
                            for t in range(2):
                                S.add("pool", lambda e, t=t: e.collective_compute(
                                    "AllGather", ALU.bypass, replica_groups=[[0, 1], [2, 3], [4, 5], [6, 7]],
                                    ins=[AOh[t].ap().opt()], outs=[AOg[t].ap().opt()]),
                                    [("AOh", h, t) for h in range(4)], [], dma=f"ccao{t}", inc=1)
                    if not dc and not (hold and not da):
                        try:
                            hold = (next(gc) == "hold")
                        except StopIteration:
                            dc = True
                S.run()

        def layer_norm(tag, r_ap, rkey, hout, hkey, gbt, gkeys, wk):
            stats, mv, nmr, mhs = wk
            for c in range(4):
                S.add("dve", lambda e, c=c: e.bn_stats(stats[:, c * 6:(c + 1) * 6], r_ap[:, c * 512:(c + 1) * 512]), [rkey], [(tag, "st", c)])
            S.add("dve", lambda e: e.bn_aggr(mv[:, 0:2], stats[:, :]), [(tag, "st", c) for c in range(4)], [(tag, "mv")])
            S.add("dve", lambda e: e.tensor_scalar_add(mv[:, 2:3], mv[:, 1:2], LN_EPS), [(tag, "mv")], [(tag, "ve")])
            S.add("pool", lambda e: e.tensor_tensor(out=mv[:, 3:4], in0=mv[:, 2:3], in1=mhs[:, 0:1], op=ALU.pow), [(tag, "ve"), "mhs"], [(tag, "rstd")])
            S.add("dve", lambda e: e.scalar_tensor_tensor(out=nmr[:, 0:1], in0=mv[:, 0:1], scalar=-1.0, in1=mv[:, 3:4], op0=ALU.mult, op1=ALU.mult),
                  [(tag, "mv"), (tag, "rstd")], [(tag, "nmr")])
            S.add("act", lambda e: e.activation(out=hout, in_=r_ap, func=AF.Identity, bias=nmr[:, 0:1], scale=mv[:, 3:4]),
                  [rkey, (tag, "nmr"), (tag, "rstd")], [hkey])
            S.add("dve", lambda e: e.tensor_tensor(out=hout, in0=hout, in1=gbt[:, 0:D], op=ALU.mult), [hkey] + gkeys, [hkey])
            S.add("dve", lambda e: e.tensor_tensor(out=hout, in0=hout, in1=gbt[:, D:2 * D], op=ALU.add), [hkey] + gkeys, [hkey])

        def stage_outproj(l):
            xsrc = x_in
            with ExitStack() as st:
                wo = sb("s4wo", [128, 16, D], BF16, st)
                wrb = sb("s4wr", [128, 16, 36], BF16, st)
                gb = sb("s4gb", [128, 2 * D], F32, st)
                rbs = sb("s4rb", [128, 36], F32, st)
                ct = [sb(f"s4ct{i}", [128, 16, 512], BF16, st) for i in range(2)]
                xt_ = [sb(f"s4x{i}", [128, D], F32, st) for i in range(2)]
                r_ = [sb(f"s4r{i}", [128, D], F32, st) for i in range(2)]
                h_ = [sb(f"s4h{i}", [128, D], F32, st) for i in range(2)]
                hb = [sb(f"s4hb{i}", [128, D], BF16, st) for i in range(2)]
                hts = [sb(f"s4ht{i}", [128, 16, 128], BF16, st) for i in range(2)]
                stats2 = [sb(f"s4stats{i}", [128, 24], F32, st) for i in range(2)]
                mv2 = [sb(f"s4mv{i}", [128, 4], F32, st) for i in range(2)]
                nmr2 = [sb(f"s4nmr{i}", [128, 1], F32, st) for i in range(2)]
                mhs = sb("s4mhs", [128, 1], F32, st)
                lg2 = [sb(f"s4lg{i}", [128, 36], F32, st) for i in range(2)]
                w12 = [sb(f"s4w1{i}", [128, 64], F32, st) for i in range(2)]
                e82 = [sb(f"s4e8{i}", [128, 32], F32, st) for i in range(2)]
                caps = []
                cmb = sb("s4cmb", [128, 8 * 32], F32, st)
                for cch in range(4):
                    for kh in range(2):
                        dma("pool", wo[:, 8 * kh:8 * kh + 8, cch * 512:(cch + 1) * 512],
                            w_out[l, 8 * kh * 128:(8 * kh + 8) * 128, cch * 512:(cch + 1) * 512].rearrange("(k p) c -> p k c", p=128),
                            f"s4wo{cch}_{kh}", [], [f"s4wo{cch}_{kh}"])
                dma("pool", wrb[:, :, :], wr[l].rearrange("(k p) c -> p k c", p=128), "s4wr", [], ["s4wr"])
                dma("sp", gb[:, 0:D], bcast_rows(lnv[l, 0:1, :], D), "s4gb0", [], ["s4gb0"])
                dma("sp", gb[:, D:2 * D], bcast_rows(lnv[l, 1:2, :], D), "s4gb1", [], ["s4gb1"])
                dma("sp", rbs[:, :], bcast_rows(rb[l:l + 1, :], 36), "s4rb", [], ["s4rb"])
                S.add("pool", lambda e: e.memset(mhs[:, :], -0.5), [], ["mhs"])
                for t in range(8):
                    c4, j = t // 4, t % 4
                    s2 = t % 2
                    c2 = c4 % 2
                    stats, mv, nmr, lg, w1, e8 = stats2[s2], mv2[s2], nmr2[s2], lg2[s2], w12[s2], e82[s2]
                    S.capture = []
                    caps.append(S.capture)
                    if j == 0:
                        dma("sp", ct[c2][:, 0:8, :],
                            (lambda r2, c4=c4: AOg[r2].ap().rearrange("(k p) t -> p k t", p=128)[:, :, c4 * 512:(c4 + 1) * 512]),
                            f"s4ct{c2}", [], [f"s4ct{c2}"])
                        dma("sp", ct[c2][:, 8:16, :], CONVT.rearrange("(k p) t -> p k t", p=128)[:, :, c4 * 512:(c4 + 1) * 512],
                            f"s4cv{c2}", [], [f"s4cv{c2}"])
                    dma("sp", xt_[s2][:, :], (own_rows(xsrc, t) if l == 0 else XSh.ap()[t * 128:(t + 1) * 128, :]), f"s4x{s2}", [], [f"s4x{s2}"])
                    for cch in range(4):
                        lst = [(bank[cch], ct[c2][:, k, j * 128:(j + 1) * 128], wo[:, k, cch * 512:(cch + 1) * 512], k == 0, k == 15) for k in range(16)]
                        mm_group(lst, [f"s4wo{cch}_0", f"s4wo{cch}_1", f"s4ct{c2}", f"s4cv{c2}"], [BK[cch]])
                    S.add("dve", lambda e, s2=s2: e.scalar_tensor_tensor(out=r_[s2][:, :], in0=xt_[s2][:, :], scalar=ALPHA, in1=pbig[:, :],
                                                                         op0=ALU.mult, op1=ALU.add),
                          [f"s4x{s2}", BK[0], BK[1], BK[2], BK[3]], [f"s4r{s2}"])
                    layer_norm(f"s4ln{s2}", r_[s2][:, :], f"s4r{s2}", h_[s2][:, :], f"s4h{s2}", gb, ["s4gb0", "s4gb1"], (stats, mv, nmr, mhs))
                    dma("sp", H[t * 128:(t + 1) * 128, :], h_[s2][:, :], f"s4h{s2}", [f"s4h{s2}"], [])
                    copy_op("act", hb[s2][:, :], h_[s2][:, :], [f"s4h{s2}"], [f"s4hb{s2}"])
                    transpose_tile(hb[s2], f"s4hb{s2}", hts[s2], f"s4ht{s2}", 0, 4, 5, t)
                    dma("sp", HT.rearrange("(k p) t -> p k t", p=128)[:, :, t * 128:(t + 1) * 128], hts[s2][:, :, :],
                        f"s4ht{s2}", [f"s4ht{s2}"], [])
                    lst = [(bank[6][:, 0:36], hts[s2][:, k, :], wrb[:, k, :], k == 0, k == 15) for k in range(16)]
                    mm_group(lst, [f"s4ht{s2}", "s4wr"], [BK[6]])
                    R = ("s4rt", s2)
                    S.add("dve", lambda e, lg=lg, w1=w1, e8=e8: e.tensor_tensor(out=lg[:, :], in0=bank[6][:, 0:36], in1=rbs[:, :], op=ALU.add), [BK[6], "s4rb"], [R])
                    S.add("dve", lambda e, lg=lg, w1=w1, e8=e8: e.tensor_reduce(out=w1[:, 0:1], in_=lg[:, 0:4], axis=AX.X, op=ALU.max), [R], [R])
                    S.add("dve", lambda e, lg=lg, w1=w1, e8=e8: e.tensor_scalar(out=w1[:, 4:8], in0=lg[:, 0:4], scalar1=w1[:, 0:1], scalar2=None, op0=ALU.is_ge), [R], [R])
                    S.add("dve", lambda e, lg=lg, w1=w1, e8=e8: e.tensor_scalar(out=w1[:, 8:12], in0=lg[:, 0:4], scalar1=w1[:, 0:1], scalar2=None, op0=ALU.subtract), [R], [R])
                    S.add("act", lambda e, lg=lg, w1=w1, e8=e8: e.activation(out=w1[:, 8:12], in_=w1[:, 8:12], func=AF.Exp, accum_out=w1[:, 1:2]), [R], [R])
                    S.add("dve", lambda e, lg=lg, w1=w1, e8=e8: e.reciprocal(w1[:, 2:3], w1[:, 1:2]), [R], [R])
                    S.add("dve", lambda e, lg=lg, w1=w1, e8=e8: e.tensor_tensor(
                        out=e8[:, :].rearrange("p (g j) -> p g j", j=8), in0=lg[:, 4:36].rearrange("p (g j) -> p g j", j=8),
                        in1=bc3(w1[:, 4:8], [[1, 4], [0, 8]]),
                        op=ALU.mult), [R], [R])
                    S.add("dve", lambda e, lg=lg, w1=w1, e8=e8: e.tensor_reduce(out=w1[:, 16:24], in_=e8[:, :].rearrange("p (g j) -> p j g", j=8), axis=AX.X, op=ALU.add), [R], [R])
                    S.add("dve", lambda e, lg=lg, w1=w1, e8=e8: e.tensor_reduce(out=w1[:, 12:13], in_=w1[:, 16:24], axis=AX.X, op=ALU.max), [R], [R])
                    S.add("dve", lambda e, lg=lg, w1=w1, e8=e8: e.tensor_scalar(out=w1[:, 24:32], in0=w1[:, 16:24], scalar1=w1[:, 12:13], scalar2=None, op0=ALU.is_ge), [R], [R])
                    S.add("dve", lambda e, lg=lg, w1=w1, e8=e8: e.scalar_tensor_tensor(out=w1[:, 32:40], in0=w1[:, 24:32], scalar=-1e30, in1=w1[:, 16:24],
                                                                  op0=ALU.mult, op1=ALU.add), [R], [R])
                    S.add("dve", lambda e, lg=lg, w1=w1, e8=e8: e.tensor_reduce(out=w1[:, 13:14], in_=w1[:, 32:40], axis=AX.X, op=ALU.max), [R], [R])
                    S.add("dve", lambda e, lg=lg, w1=w1, e8=e8: e.tensor_scalar(out=w1[:, 40:48], in0=w1[:, 32:40], scalar1=w1[:, 13:14], scalar2=None, op0=ALU.is_ge), [R], [R])
                    S.add("dve", lambda e, lg=lg, w1=w1, e8=e8: e.tensor_tensor(out=w1[:, 14:15], in0=w1[:, 13:14], in1=w1[:, 12:13], op=ALU.subtract), [R], [R])
                    S.add("act", lambda e, lg=lg, w1=w1, e8=e8: e.activation(out=w1[:, 14:15], in_=w1[:, 14:15], func=AF.Exp), [R], [R])
                    S.add("dve", lambda e, lg=lg, w1=w1, e8=e8: e.tensor_scalar_add(w1[:, 15:16], w1[:, 14:15], 1.0), [R], [R])
                    S.add("dve", lambda e, lg=lg, w1=w1, e8=e8: e.reciprocal(w1[:, 15:16], w1[:, 15:16]), [R], [R])
                    S.add("dve", lambda e, lg=lg, w1=w1, e8=e8: e.tensor_tensor(out=w1[:, 48:49], in0=w1[:, 15:16], in1=w1[:, 2:3], op=ALU.mult), [R], [R])
                    S.add("dve", lambda e, lg=lg, w1=w1, e8=e8: e.tensor_tensor(out=w1[:, 49:50], in0=w1[:, 48:49], in1=w1[:, 14:15], op=ALU.mult), [R], [R])
                    S.add("dve", lambda e, lg=lg, w1=w1, e8=e8: e.tensor_scalar(out=w1[:, 24:32], in0=w1[:, 24:32], scalar1=w1[:, 48:49], scalar2=None, op0=ALU.mult), [R], [R])
                    S.add("dve", lambda e, lg=lg, w1=w1, e8=e8: e.scalar_tensor_tensor(out=w1[:, 24:32], in0=w1[:, 40:48], scalar=w1[:, 49:50], in1=w1[:, 24:32],
                                                                  op0=ALU.mult, op1=ALU.add), [R], [R])
                    S.add("dve", lambda e, t=t, lg=lg, w1=w1, e8=e8: e.tensor_tensor(
                        out=cmb[:, t * 32:(t + 1) * 32].rearrange("p (g j) -> p g j", j=8),
                        in0=bc3(w1[:, 4:8], [[1, 4], [0, 8]]),
                        in1=bc3(w1[:, 24:32], [[0, 4], [1, 8]]),
                        op=ALU.mult), [R], [R, ("s4cmb", t)])
                    S.capture = None
                for t0 in range(0, 8, 2):
                    S.add_interleaved(caps[t0], caps[t0 + 1])

                dma("sp", COMB.rearrange("(t p) e -> p t e", p=128), cmb[:, :].rearrange("p (t e) -> p t e", e=32), "s4cmb",
                    [("s4cmb", t) for t in range(8)], [])
                S.run()

        def stage_moe(l, acc):
            t0 = 0
            with ExitStack() as st:
                hT = sb("s5hT", [128, 16, 1024], BF16, st)
                cb = sb("s5cb", [128, 8 * 32], F32, st)
                wg = [sb(f"s5wg{i}", [128, 16, 512], BF16, st) for i in range(3)]
                wd = [sb(f"s5wd{i}", [128, 2, D], BF16, st) for i in range(3)]
                sg = [sb(f"s5sg{i}", [128, 256], F32, st) for i in range(2)]
                ab = [sb(f"s5ab{i}", [128, 256], BF16, st) for i in range(2)]
                aT = [sb(f"s5aT{i}", [128, 256], BF16, st) for i in range(2)]
                for k in range(16):
                    dma("sp", hT[:, k, :], HT[k * 128:(k + 1) * 128, t0:t0 + 1024], f"s5hT{k}", [], [f"s5hT{k}"])
                hkeys = [f"s5hT{k}" for k in range(16)]
                dma("sp", cb[:, :].rearrange("p (t e) -> p t e", e=32), COMB[t0:t0 + 1024, :].rearrange("(t p) e -> p t e", p=128), "s5cb", [], ["s5cb"])
                def load_e(ex):
                    s3 = ex % 3
                    for j in range(4):
                        dma("pool", wg[s3][:, 4 * j:4 * j + 4, :], w_gu[l, ex, 4 * j * 128:(4 * j + 4) * 128, :].rearrange("(k p) c -> p k c", p=128),
                            f"s5wg{s3}_{j}", [], [f"s5wg{s3}_{j}"])
                    dma("pool", wd[s3][:, :, :], w_dn[l, ex].rearrange("(k p) c -> p k c", p=128), f"s5wd{s3}", [], [f"s5wd{s3}"])
                load_e(0)
                load_e(1)
                for j in range(8):
                    dma("sp", acc[:, j, :], H[j * 128:(j + 1) * 128, :], f"s5acc{j}", [], [("acc", j)])
                    S.add("dve", lambda e, j=j: e.tensor_scalar(out=acc[:, j, :], in0=acc[:, j, :], scalar1=ALPHA, scalar2=None, op0=ALU.mult),
                          [("acc", j)], [("acc", j)])
                for ex in range(32):
                    if ex + 2 < 32:
                        load_e(ex + 2)
                    s3 = ex % 3
                    gkeys = [f"s5wg{s3}_{j}" for j in range(4)]

                    def hid(i):
                        b = 4 + i % 2
                        lst = [(bank[b], hT[:, k, i * 128:(i + 1) * 128], wg[s3][:, k, :], k == 0, k == 15) for k in range(16)]
                        mm_group(lst, gkeys + hkeys, [BK[b]])
                        S.add("act", lambda e, b=b, i=i: e.activation(out=sg[i % 2][:, :], in_=bank[b][:, 0:256], func=AF.Silu), [BK[b]], [f"s5sg{i % 2}"])
                        S.add("dve", lambda e, b=b, i=i, ex=ex: e.scalar_tensor_tensor(
                            out=ab[i % 2][:, :], in0=bank[b][:, 256:512], scalar=cb[:, i * 32 + ex:i * 32 + ex + 1], in1=sg[i % 2][:, :],
                            op0=ALU.mult, op1=ALU.mult), [BK[b], f"s5sg{i % 2}", "s5cb"], [f"s5ab{i % 2}"])

                    def trp(j):
                        lst = [(bank_bf[6][:, (j % 2) * 256 + f * 128:(j % 2) * 256 + (f + 1) * 128], ab[j % 2][:, f * 128:(f + 1) * 128]) for f in range(2)]
                        tr_group(lst, [f"s5ab{j % 2}"], [("b6h", j % 2)])
                        copy_op("act", aT[j % 2][:, :], bank_bf[6][:, (j % 2) * 256:(j % 2) * 256 + 256], [("b6h", j % 2)], [f"s5aT{j % 2}"])

                    def down(j):
                        lst = []
                        for cch in range(4):
                            lst += [(bank[cch], aT[j % 2][:, f * 128:(f + 1) * 128], wd[s3][:, f, cch * 512:(cch + 1) * 512], f == 0, f == 1) for f in range(2)]
                        mm_group(lst, [f"s5aT{j % 2}", f"s5wd{s3}"], [BK[0], BK[1], BK[2], BK[3]])
                        S.add("dve", lambda e, j=j: e.tensor_tensor(out=acc[:, j, :], in0=acc[:, j, :], in1=pbig[:, :], op=ALU.add),
                              [("acc", j), BK[0], BK[1], BK[2], BK[3]], [("acc", j)])
                    hid(0)
                    hid(1)
                    for j in range(8):
                        trp(j)
                        if j + 2 < 8:
                            hid(j + 2)
                        down(j)
                S.run()

        def stage_ple(l, acc):
            last = (l == n_layers - 1)
            with ExitStack() as st:
                wgt = sb("s6wg", [128, 16, D], BF16, st)
                wpl = sb("s6wp", [128, 2, D], BF16, st)
                gb = sb("s6gb", [128, 2 * D], F32, st)
                h2b = sb("s6hb", [128, D], BF16, st)
                h2T = sb("s6hT", [128, 16, 128], BF16, st)
                pt_ = [sb(f"s6p{i}", [128, 256], F32, st) for i in range(2)]
                ptb = sb("s6pb", [128, 256], BF16, st)
                pT = sb("s6pT", [128, 256], BF16, st)
                sgm = sb("s6sg", [128, D], F32, st)
                xnb = sb("s6xb", [128, D], BF16, st)
                xts = [sb(f"s6xt{i}", [128, 16, 128], BF16, st) for i in range(2)]
                stats2 = [sb(f"s6stats{i}", [128, 24], F32, st) for i in range(2)]
                mv2 = [sb(f"s6mv{i}", [128, 4], F32, st) for i in range(2)]
                nmr2 = [sb(f"s6nmr{i}", [128, 1], F32, st) for i in range(2)]
                caps = []
                mhs = sb("s6mhs", [128, 1], F32, st)
                for cch in range(4):
                    for kh in range(2):
                        dma("pool", wgt[:, 8 * kh:8 * kh + 8, cch * 512:(cch + 1) * 512],
                            ple_g[l, 8 * kh * 128:(8 * kh + 8) * 128, cch * 512:(cch + 1) * 512].rearrange("(k p) c -> p k c", p=128),
                            f"s6wg{cch}_{kh}", [], [f"s6wg{cch}_{kh}"])
                dma("pool", wpl[:, :, :], ple_w[l].rearrange("(k p) c -> p k c", p=128), "s6wp", [], ["s6wp"])
                dma("sp", gb[:, 0:D], bcast_rows(lnv[l, 2:3, :], D), "s6gb0", [], ["s6gb0"])
                dma("sp", gb[:, D:2 * D], bcast_rows(lnv[l, 3:4, :], D), "s6gb1", [], ["s6gb1"])
                S.add("pool", lambda e: e.memset(mhs[:, :], -0.5), [], ["mhs"])
                PB4 = [BK[0], BK[1], BK[2], BK[3]]
                for j in range(8):
                    t = j
                    s2 = j % 2
                    stats, mv, nmr = stats2[s2], mv2[s2], nmr2[s2]
                    S.capture = []
                    caps.append(S.capture)
                    ak = ("acc", j)
                    at = acc[:, j, :]
                    dma("sp", pt_[s2][:, :], p_in[l, j * 128:(j + 1) * 128, :], f"s6p{s2}", [], [f"s6p{s2}"])
                    layer_norm(f"s6ln{s2}", at, ak, at, ak, gb, ["s6gb0", "s6gb1"], (stats, mv, nmr, mhs))
                    copy_op("act", h2b[:, :], at, [ak], ["s6hb"])
                    transpose_tile(h2b, "s6hb", h2T, "s6hT", 0, 4, 5, j)
                    for cch in range(4):
                        lst = [(bank[cch], h2T[:, k, :], wgt[:, k, cch * 512:(cch + 1) * 512], k == 0, k == 15) for k in range(16)]
                        mm_group(lst, [f"s6wg{cch}_0", f"s6wg{cch}_1", "s6hT"], [BK[cch]])
                    S.add("act", lambda e: e.activation(out=sgm[:, :], in_=pbig[:, :], func=AF.Sigmoid), PB4, ["s6sg"])
                    copy_op("dve", ptb[:, :], pt_[s2][:, :], [f"s6p{s2}"], ["s6pb"])
                    tr_group([(bank_bf[6][:, f * 128:(f + 1) * 128], ptb[:, f * 128:(f + 1) * 128]) for f in range(2)], ["s6pb"], [BK[6]])
                    copy_op("dve", pT[:, :], bank_bf[6][:, 0:256], [BK[6]], ["s6pT"])
                    lst = []
                    for cch in range(4):
                        lst += [(bank[cch], pT[:, f * 128:(f + 1) * 128], wpl[:, f, cch * 512:(cch + 1) * 512], f == 0, f == 1) for f in range(2)]
                    mm_group(lst, ["s6pT", "s6wp"], PB4)
                    S.add("dve", lambda e: e.tensor_tensor(out=sgm[:, :], in0=sgm[:, :], in1=pbig[:, :], op=ALU.mult), ["s6sg"] + PB4, ["s6sg"])
                    S.add("dve", lambda e, at=at: e.tensor_tensor(out=at, in0=sgm[:, :], in1=at, op=ALU.add), ["s6sg", ak], [ak])
                    if last:
                        dma("sp", out[t * 128:(t + 1) * 128, :], at, "s6xo", [ak], [])
                    else:
                        dma("sp", XSh.ap()[t * 128:(t + 1) * 128, :], at, "s6xo", [ak], [])
                        copy_op("act", xnb[:, :], at, [ak], ["s6xb"])
                        transpose_tile(xnb, "s6xb", xts[s2], f"s6xt{s2}", 0, 6, 7, j)
                        for c in range(4):
                            dma("sp", XTh[c].ap().rearrange("(k p) t -> p k t", p=128)[:, :, t * 128:(t + 1) * 128], xts[s2][:, 4 * c:4 * c + 4, :],
                                f"s6xt{s2}_{c}", [f"s6xt{s2}"], [])
                    S.capture = None
                for t0 in range(0, 8, 2):
                    S.add_interleaved(caps[t0], caps[t0 + 1])
                S.run()

        stage_x_to_xt()
        for l in range(n_layers):
            if upto >= 1:
                stage_inproj(l)
            if upto >= 2:
                stage_attn_conv(l)
            if upto >= 4:
                stage_outproj(l)
            if upto >= 5:
                with nc.sbuf_tensor(f"acc_{l}", [128, 8, D], F32) as acc:
                    stage_moe(l, acc)
                    if upto >= 6:
                        stage_ple(l, acc)
                if upto >= 6 and l < n_layers - 1:
                    with nc.Block() as block:
                        @block.gpsimd
                        def _(g):
                            for (src, dst) in zip(XTh, XTg):
                                ccn[0] += 1
                                g.collective_compute("AllGather", ALU.bypass, replica_groups=[[0, 1], [2, 3], [4, 5], [6, 7]],
                                                     ins=[src.ap().opt()], outs=[dst.ap().opt()]).then_inc(ccsem)
                                g.wait_ge(ccsem, ccn[0])
    return nc


def t5_bucket_np(rel):
    n = np.maximum(rel, 0)
    nf = np.maximum(n, 16).astype(np.float32)
    large = 16 + (np.log(nf / np.float32(16)) / np.float32(np.log(128 / 16)) * np.float32(16)).astype(np.int32)
    large = np.minimum(large, 31)
    return np.where(n < 16, n, large)


def make_consts():
    c = np.zeros((128, C_W), np.float32)
    c[:, C_ID:C_ID + 128] = np.eye(128, dtype=np.float32)
    c[:, C_ONE:C_ONE + 128] = 1.0
    for n in range(8):
        c[n, C_SEL + n * 128:C_SEL + (n + 1) * 128] = 1.0
    k = np.arange(128)[:, None]
    i = np.arange(128)[None, :]
    c[:, C_CM:C_CM + 128] = np.where(i < k, NEG, 0.0)
    for ti in range(8):
        J = 4 + ti // 2
        for n in range(8):
            c[:, C_PM + ti * 8 + n] = 0.0 if n < J else -1e30
    return c


def host_inputs(inputs):
    f = lambda a: np.ascontiguousarray(np.asarray(a, dtype=np.float32))
    rel_bias = f(inputs["rel_bias"])
    k = np.arange(128)[:, None]
    i = np.arange(256)[None, :]
    bidx = t5_bucket_np(i - k)
    braw = rel_bias[bidx, :].transpose(0, 2, 1)
    c31f = np.broadcast_to(rel_bias[31:32, :], (128, 8))
    biasraw = [np.ascontiguousarray(braw[:, 4 * r:4 * r + 4, :].reshape(128, 4 * 256)) for r in range(2)]
    c31 = [np.ascontiguousarray(c31f[:, 4 * r:4 * r + 4]) for r in range(2)]
    conv_w = f(inputs["conv_w"])
    convw = np.ascontiguousarray(conv_w.reshape(4, 31, 8, 128).transpose(0, 3, 2, 1).reshape(4, 128, 8 * 31))
    cv = np.stack([f(inputs["conv_b"]), f(inputs["conv_ln_g"]), f(inputs["conv_ln_b"])], axis=1)
    convv = np.ascontiguousarray(cv.reshape(4, 3, 8, 128).transpose(0, 3, 1, 2).reshape(4, 128, 24))
    lnv = np.ascontiguousarray(np.stack([f(inputs["ln1_g"]), f(inputs["ln1_b"]), f(inputs["ln2_g"]), f(inputs["ln2_b"])], axis=1))
    wr = np.ascontiguousarray(np.concatenate([f(inputs["router_g_w"]), f(inputs["router_e_w"])], axis=2))
    rb = np.ascontiguousarray(np.concatenate([f(inputs["router_g_b"]), f(inputs["router_e_b"])], axis=1))
    shared = {
        "w_in": f(inputs["w_in"]), "convw": convw, "convv": convv, "w_out": f(inputs["w_out"]),
        "biasraw": biasraw, "c31": c31, "lnv": lnv, "wr": wr, "rb": rb,
        "w_gu": f(inputs["expert_w_gu"]), "w_dn": f(inputs["expert_w_down"]),
        "ple_w": f(inputs["ple_w"]), "ple_g": f(inputs["ple_gate_w"]), "cst": make_consts(),
    }
    return shared


_NC_CACHE = {}


def kernel(**inputs):
    x = np.asarray(inputs["x"], dtype=np.float32)
    p = np.asarray(inputs["p"], dtype=np.float32)
    B = x.shape[0]
    shared = host_inputs(inputs)
    if "nc" not in _NC_CACHE:
        _NC_CACHE["nc"] = build()
    nc = _NC_CACHE["nc"]
    in_maps = []
    for b in range(B):
        for r in range(2):
            m = dict(shared)
            m["x"] = np.ascontiguousarray(x[b])
            m["p"] = np.ascontiguousarray(p[:, b, r * NH:(r + 1) * NH])
            m["biasraw"] = shared["biasraw"][r]
            m["c31"] = shared["c31"][r]
            in_maps.append(m)
    res = run_bass_kernel_spmd(nc, in_maps, core_ids=list(range(2 * B)))
    outs = [np.asarray(r["out"], dtype=np.float32) for r in res.results]
    return np.stack([np.concatenate([outs[2 * b], outs[2 * b + 1]], axis=0) for b in range(B)], axis=0)
```
